# Optimizing a Trainium2 kernel written in Bass

```python
import math
import jax, jax.numpy as jnp
from jax import lax
import numpy as np

D_MODEL = 1024
BATCH = 8
SEQ = 4096
DEPTH = 4

HEAD_DIM = 64
SWA_HEADS = 8
SWA_KV_HEADS = 2
SWA_GROUP = SWA_HEADS // SWA_KV_HEADS
WINDOW = 128
ATTN_BLOCK = 128
HG_HEADS = 4
HG_DK = 64
HG_DV = 64
HG_CHUNK = 16
MEM_LEN = 256
MEM_HEADS = 4
N_BUCKETS = 32
MAX_DISTANCE = 128
N_EXPERTS = 16
N_GROUPS = 4
EXPERTS_PER_GROUP = N_EXPERTS // N_GROUPS
TOP_K = 2
D_EXPERT = 512
MOE_BLOCK = 256
LN_EPS = 1e-5
RMS_EPS = 1e-6
ALPHA = (2 * DEPTH) ** 0.25
BETA = (8 * DEPTH) ** -0.25
SPLITS = (SWA_HEADS * HEAD_DIM, SWA_KV_HEADS * HEAD_DIM, SWA_KV_HEADS * HEAD_DIM,
          HG_HEADS * HG_DK, HG_HEADS * HG_DK, HG_HEADS * HG_DV, HG_HEADS * HG_DV,
          MEM_HEADS * HEAD_DIM)
IN_WIDTH = sum(SPLITS)
MIX_WIDTH = SWA_HEADS * HEAD_DIM + HG_HEADS * HG_DV + MEM_HEADS * HEAD_DIM

kernel_name = "hybrid_swa_hgrn2_memory_moe_deepnorm"


def layer_norm(x, g, b):
    xf = x.astype(jnp.float32)
    mu = jnp.mean(xf, axis=-1, keepdims=True)
    var = jnp.mean(jnp.square(xf - mu), axis=-1, keepdims=True)
    return ((xf - mu) * lax.rsqrt(var + LN_EPS) * g.astype(jnp.float32) + b.astype(jnp.float32)).astype(x.dtype)


def t5_bucket(dist):
    max_exact = N_BUCKETS // 2
    d = jnp.maximum(dist, 0)
    large = max_exact + (jnp.log(jnp.maximum(d, 1).astype(jnp.float32) / max_exact)
                         / math.log(MAX_DISTANCE / max_exact) * (N_BUCKETS - max_exact)).astype(jnp.int32)
    large = jnp.minimum(large, N_BUCKETS - 1)
    return jnp.where(d < max_exact, d, large)


def banded_rel_bias(rel_bias):
    i = jnp.arange(ATTN_BLOCK)[:, None]
    j = jnp.arange(2 * ATTN_BLOCK)[None, :]
    bias = rel_bias[t5_bucket(i + ATTN_BLOCK - j)]
    return jnp.transpose(bias, (2, 0, 1)).astype(jnp.float32)


def swa_with_sinks(q, k, v, sinks, pos_bias):
    B, S, _ = q.shape
    nb = S // ATTN_BLOCK
    qb = q.reshape(B, nb, ATTN_BLOCK, SWA_KV_HEADS, SWA_GROUP, HEAD_DIM)

    def with_prev(t):
        tb = t.reshape(B, nb, ATTN_BLOCK, SWA_KV_HEADS, HEAD_DIM)
        prev = jnp.pad(tb, ((0, 0), (1, 0), (0, 0), (0, 0), (0, 0)))[:, :-1]
        return jnp.concatenate([prev, tb], axis=2)

    kb, vb = with_prev(k), with_prev(v)
    logits = jnp.einsum('bnqhgd,bnkhd->bnhgqk', qb, kb).astype(jnp.float32) * (HEAD_DIM ** -0.5)
    logits = logits + pos_bias.reshape(SWA_KV_HEADS, SWA_GROUP, ATTN_BLOCK, 2 * ATTN_BLOCK)
    i = jnp.arange(ATTN_BLOCK)[:, None]
    j = jnp.arange(2 * ATTN_BLOCK)[None, :]
    dist = i + ATTN_BLOCK - j
    in_window = (dist >= 0) & (dist < WINDOW)
    not_pad = (jnp.arange(nb)[:, None, None] > 0) | (j >= ATTN_BLOCK)[None]
    mask = in_window[None] & not_pad
    logits = jnp.where(mask[None, :, None, None], logits, -jnp.inf)
    sink = jnp.broadcast_to(sinks.astype(jnp.float32).reshape(SWA_KV_HEADS, SWA_GROUP, 1, 1),
                            logits.shape[:-1] + (1,))
    probs = jax.nn.softmax(jnp.concatenate([logits, sink], axis=-1), axis=-1)[..., :-1]
    out = jnp.einsum('bnhgqk,bnkhd->bnqhgd', probs.astype(v.dtype), vb)
    return out.reshape(B, S, SWA_HEADS * HEAD_DIM)


def hgrn2(q, f, i, gate, lb, norm_w):
    B, S, _ = q.shape
    nc = S // HG_CHUNK
    f32 = jnp.float32

    def heads(t, d):
        return t.astype(f32).reshape(B, nc, HG_CHUNK, HG_HEADS, d).transpose(0, 3, 1, 2, 4)

    lbf = lb.astype(f32).reshape(HG_HEADS, 1, 1, HG_DK)
    log_f = jnp.logaddexp(jnp.log(lbf), jnp.log1p(-lbf) + jax.nn.log_sigmoid(heads(f, HG_DK)))
    kk = -jnp.expm1(log_f)
    qq = jax.nn.silu(heads(q, HG_DK)) * (HG_DK ** -0.5)
    vv = heads(i, HG_DV)
    b = jnp.cumsum(log_f, axis=3)
    causal = jnp.tril(jnp.ones((HG_CHUNK, HG_CHUNK), dtype=bool))
    diff = b[..., :, None, :] - b[..., None, :, :]
    decay = jnp.exp(jnp.where(causal[:, :, None], diff, -jnp.inf))
    scores = jnp.einsum('bhntk,bhnsk,bhntsk->bhnts', qq, kk, decay)
    o_intra = jnp.einsum('bhnts,bhnsv->bhntv', scores, vv)
    b_last = b[..., -1:, :]
    u = jnp.einsum('bhnsk,bhnsv->bhnkv', kk * jnp.exp(b_last - b), vv)
    chunk_decay = jnp.exp(b_last[..., 0, :])

    def step(state, inp):
        dec, upd = inp
        return dec[..., None] * state + upd, state

    s0 = jnp.zeros((B, HG_HEADS, HG_DK, HG_DV), f32)
    _, s_prev = lax.scan(step, s0, (jnp.moveaxis(chunk_decay, 2, 0), jnp.moveaxis(u, 2, 0)))
    s_prev = jnp.moveaxis(s_prev, 0, 2)
    o_inter = jnp.einsum('bhntk,bhnkv->bhntv', qq * jnp.exp(b), s_prev)
    o = (o_intra + o_inter).transpose(0, 2, 3, 1, 4).reshape(B, S, HG_HEADS, HG_DV)
    o = o * lax.rsqrt(jnp.mean(o * o, axis=-1, keepdims=True) + RMS_EPS) * norm_w.astype(f32)
    o = o.reshape(B, S, HG_HEADS * HG_DV) * jax.nn.silu(gate.astype(f32))
    return o.astype(q.dtype)


def memory_attention(q, mem_k, mem_v):
    B, S, _ = q.shape
    qh = q.reshape(B, S, MEM_HEADS, HEAD_DIM)
    logits = jnp.einsum('bshd,bmhd->bhsm', qh, mem_k).astype(jnp.float32) * (HEAD_DIM ** -0.5)
    probs = jax.nn.softmax(logits, axis=-1)
    out = jnp.einsum('bhsm,bmhd->bshd', probs.astype(mem_v.dtype), mem_v)
    return out.reshape(B, S, MEM_HEADS * HEAD_DIM)


def route(x2, w_router, router_bias):
    scores = jax.nn.sigmoid((x2 @ w_router).astype(jnp.float32))
    sel = scores + router_bias.astype(jnp.float32)
    grouped = sel.reshape(-1, N_GROUPS, EXPERTS_PER_GROUP)
    group_score = jnp.sum(lax.top_k(grouped, TOP_K)[0], axis=-1)
    best = jnp.argmax(group_score, axis=-1)
    in_group = (jnp.arange(N_EXPERTS) // EXPERTS_PER_GROUP)[None, :] == best[:, None]
    _, idx = lax.top_k(jnp.where(in_group, sel, -jnp.inf), TOP_K)
    w = jnp.take_along_axis(scores, idx, axis=-1)
    w = w / jnp.sum(w, axis=-1, keepdims=True)
    return idx, w


def moe(x, w_router, router_bias, w_gate, w_up, w_down):
    B, S, D = x.shape
    n_tok = B * S
    x2 = x.reshape(n_tok, D)
    idx, w = route(x2, w_router, router_bias)
    n_assign = n_tok * TOP_K
    e_flat = idx.reshape(-1)
    tok = jnp.repeat(jnp.arange(n_tok, dtype=jnp.int32), TOP_K)
    w_flat = w.reshape(-1)
    order = jnp.argsort(e_flat)
    e_sorted = e_flat[order]
    counts = jnp.bincount(e_flat, length=N_EXPERTS)
    padded = (counts + MOE_BLOCK - 1) // MOE_BLOCK * MOE_BLOCK
    starts = jnp.cumsum(counts) - counts
    padded_ends = jnp.cumsum(padded)
    padded_starts = padded_ends - padded
    dest = padded_starts[e_sorted] + jnp.arange(n_assign) - starts[e_sorted]
    cap = n_assign + N_EXPERTS * MOE_BLOCK
    n_blocks = cap // MOE_BLOCK
    slot_tok = jnp.zeros((cap,), jnp.int32).at[dest].set(tok[order])
    slot_w = jnp.zeros((cap,), w_flat.dtype).at[dest].set(w_flat[order])
    block_expert = jnp.minimum(
        jnp.searchsorted(padded_ends, jnp.arange(n_blocks) * MOE_BLOCK, side='right'), N_EXPERTS - 1)
    xs = x2[slot_tok].reshape(n_blocks, MOE_BLOCK, D)

    def expert_block(args):
        xb, e = args
        h = jax.nn.silu(xb @ w_gate[e]) * (xb @ w_up[e])
        return h @ w_down[e]

    ys = lax.map(expert_block, (xs, block_expert)).reshape(cap, D)
    y = jnp.zeros((n_tok, D), x.dtype).at[slot_tok].add(ys * slot_w[:, None].astype(ys.dtype))
    return y.reshape(B, S, D)


def setup_inputs(seed: int = 0) -> dict:
    key = jax.random.key(seed)
    ks = jax.random.split(key, 20)

    def normal(k, shape, scale):
        return jax.random.normal(k, shape, jnp.float32) * scale

    col_scale = jnp.concatenate([jnp.full((wd,), s, jnp.float32) for wd, s in
                                 zip(SPLITS, (1.0, 1.0, BETA, 1.0, 1.0, BETA, 1.0, 1.0))])
    mem_scale = jnp.concatenate([jnp.ones((MEM_HEADS * HEAD_DIM,), jnp.float32),
                                 jnp.full((MEM_HEADS * HEAD_DIM,), BETA, jnp.float32)])
    return {
        "x": normal(ks[0], (BATCH, SEQ, D_MODEL), 1.0),
        "mem": normal(ks[1], (BATCH, MEM_LEN, D_MODEL), 1.0),
        "w_in": normal(ks[2], (DEPTH, D_MODEL, IN_WIDTH), D_MODEL ** -0.5) * col_scale,
        "b_in": normal(ks[3], (DEPTH, IN_WIDTH), 0.02),
        "w_mem_kv": normal(ks[4], (DEPTH, D_MODEL, 2 * MEM_HEADS * HEAD_DIM), D_MODEL ** -0.5) * mem_scale,
        "attn_sinks": normal(ks[5], (DEPTH, SWA_HEADS), 0.5),
        "rel_bias": normal(ks[6], (N_BUCKETS, SWA_HEADS), 0.5),
        "hgrn_lb_logits": normal(ks[7], (DEPTH, HG_HEADS * HG_DK), 1.0),
        "hgrn_norm": 1.0 + normal(ks[8], (DEPTH, HG_DV), 0.02),
        "w_out": normal(ks[9], (DEPTH, MIX_WIDTH, D_MODEL), MIX_WIDTH ** -0.5 * BETA),
        "ln1_g": 1.0 + normal(ks[10], (DEPTH, D_MODEL), 0.02),
        "ln1_b": normal(ks[11], (DEPTH, D_MODEL), 0.02),
        "w_router": normal(ks[12], (D_MODEL, N_EXPERTS), D_MODEL ** -0.5),
        "router_bias": normal(ks[13], (N_EXPERTS,), 0.01),
        "w_gate": normal(ks[14], (DEPTH, N_EXPERTS, D_MODEL, D_EXPERT), D_MODEL ** -0.5),
        "w_up": normal(ks[15], (DEPTH, N_EXPERTS, D_MODEL, D_EXPERT), D_MODEL ** -0.5),
        "w_down": normal(ks[16], (DEPTH, N_EXPERTS, D_EXPERT, D_MODEL), D_EXPERT ** -0.5 * BETA),
        "ln2_g": 1.0 + normal(ks[17], (DEPTH, D_MODEL), 0.02),
        "ln2_b": normal(ks[18], (DEPTH, D_MODEL), 0.02),
    }


def reference(x, mem, w_in, b_in, w_mem_kv, attn_sinks, rel_bias, hgrn_lb_logits, hgrn_norm, w_out,
              ln1_g, ln1_b, w_router, router_bias, w_gate, w_up, w_down, ln2_g, ln2_b):
    B, S, _ = x.shape
    M = mem.shape[1]
    split_points = [int(c) for c in np.cumsum(SPLITS)[:-1]]
    pos_bias = banded_rel_bias(rel_bias)
    lb_all = jnp.cumsum(jax.nn.softmax(hgrn_lb_logits.astype(jnp.float32), axis=0), axis=0)
    lb_all = lb_all - lb_all[0:1]
    for l in range(DEPTH):
        proj = x @ w_in[l] + b_in[l]
        sq, sk, sv, hq, hf, hi, hg, mq = jnp.split(proj, split_points, axis=-1)
        mk, mv = jnp.split(mem @ w_mem_kv[l], 2, axis=-1)
        mk = mk.reshape(B, M, MEM_HEADS, HEAD_DIM)
        mv = mv.reshape(B, M, MEM_HEADS, HEAD_DIM)
        mix = jnp.concatenate([
            swa_with_sinks(sq, sk, sv, attn_sinks[l], pos_bias),
            hgrn2(hq, hf, hi, hg, lb_all[l], hgrn_norm[l]),
            memory_attention(mq, mk, mv),
        ], axis=-1)
        x = layer_norm(ALPHA * x + mix @ w_out[l], ln1_g[l], ln1_b[l])
        x = layer_norm(ALPHA * x + moe(x, w_router, router_bias, w_gate[l], w_up[l], w_down[l]),
                       ln2_g[l], ln2_b[l])
    return x
```

```python
import math
import numpy as np
from contextlib import ExitStack
import concourse.bass as bass
import concourse.mybir as mybir
from concourse.bass_utils import run_bass_kernel_spmd

F32 = mybir.dt.float32
BF16 = mybir.dt.bfloat16
AF = mybir.ActivationFunctionType
ALU = mybir.AluOpType
AX = mybir.AxisListType

L = 4
D = 1024
S = 4096
NB = S // 128
MEM = 256
NE = 16
DE = 512
NCORES = 8
ALPHA = (2 * L) ** 0.25
LN_EPS = 1e-5
RMS_EPS = 1e-6
NFM = 13
TMW = 384
ENGINES = ["sync", "scalar", "vector", "gpsimd", "tensor"]

SAME_ENGINE_SYNC = True
SYNC_LAT = 250.0
TSUP = 1024
NJ = (S + 4 * 511) // 512
NSLOT = NJ * 512
XW = 1040
I32 = mybir.dt.int32
NBS = TSUP // 128


class Prog:
    def __init__(self):
        self.ops = {e: [] for e in ENGINES}
        self.cnt = {e: 0 for e in ENGINES}
        self.known = {e: {} for e in ENGINES}
        self.bufs = {}
        self.dma_pools = {}
        self.dma_rr = {}
        self.dma_cnt = {}
        self.enabled = True
        self.cap = None

    def add_pool(self, name, n):
        self.dma_pools[name] = [f"d_{name}{i}" for i in range(n)]
        self.dma_rr[name] = 0
        for k in self.dma_pools[name]:
            self.dma_cnt[k] = 0

    def _deps(self, eng, reads, writes):
        need = {}

        def add(tok):
            if tok is None:
                return
            k, v = tok
            if need.get(k, 0) < v:
                need[k] = v
        for b in reads:
            st = self.bufs.get(b)
            if st:
                add(st[0])
        for b in writes:
            st = self.bufs.get(b)
            if st:
                add(st[0])
                for t in st[1].values():
                    add(t)
        waits = []
        for k, v in need.items():
            if k == eng and (eng == "tensor" or not SAME_ENGINE_SYNC):
                continue
            if self.known[eng].get(k, 0) >= v:
                continue
            self.known[eng][k] = v
            waits.append((k, v))
        return waits

    def _commit(self, tok, reads, writes):
        for b in reads:
            st = self.bufs.setdefault(b, [None, {}])
            st[1][tok[0]] = tok
        for b in writes:
            self.bufs[b] = [tok, {}]

    def op(self, eng, fn, reads=(), writes=(), cost=200.0):
        if not self.enabled:
            return
        if self.cap is not None:
            self.cap.append(("op", eng, fn, list(reads), list(writes), cost, None))
            return
        ex = [b for b in reads if isinstance(b, tuple) and len(b) == 2 and b[0] == "ps"]
        if ex:
            reads = [b for b in reads if b not in ex]
            writes = list(writes) + ex
        waits = self._deps(eng, reads, writes)
        self.cnt[eng] += 1
        tok = (eng, self.cnt[eng])
        self.ops[eng].append((waits, fn, (eng, 1)))
        self._commit(tok, reads, writes)

    def dma(self, q, out, in_, reads, writes, pool, fn=None, cost=4000.0):
        if not self.enabled:
            return
        if self.cap is not None:
            self.cap.append(("dma", q, fn, list(reads), list(writes), cost, (out, in_, pool)))
            return
        sems = self.dma_pools[pool]
        i = self.dma_rr[pool]
        self.dma_rr[pool] = (i + 1) % len(sems)
        sk = sems[i]
        prev = self.dma_cnt[sk]
        waits = self._deps(q, reads, writes)
        if prev > 0 and self.known[q].get(sk, 0) < prev:
            self.known[q][sk] = prev
            waits.append((sk, prev))
        self.dma_cnt[sk] = prev + 16
        tok = (sk, prev + 16)
        if fn is None:
            fn = (lambda e, o=out, a=in_: e.dma_start(out=o, in_=a))
        self.ops[q].append((waits, fn, (sk, 16)))
        self._commit(tok, reads, writes)

    def begin_capture(self):
        self.cap = []

    def end_capture(self, sync_lat=None):
        sync_lat = SYNC_LAT if sync_lat is None else sync_lat
        ops, self.cap = self.cap, None
        n = len(ops)
        last_w, readers = {}, {}
        preds = [set() for _ in range(n)]
        for i, (kind, eng, fn, reads, writes, cost, extra) in enumerate(ops):
            ex = [b for b in reads if isinstance(b, tuple) and len(b) == 2 and b[0] == "ps"]
            rd = [b for b in reads if b not in ex]
            wr = list(writes) + ex
            for b in rd:
                if b in last_w:
                    preds[i].add(last_w[b])
            for b in wr:
                if b in last_w:
                    preds[i].add(last_w[b])
                for r_ in readers.get(b, ()):
                    preds[i].add(r_)
            for b in rd:
                readers.setdefault(b, []).append(i)
            for b in wr:
                last_w[b] = i
                readers[b] = []
            preds[i].discard(i)
        succs = [[] for _ in range(n)]
        npred = [len(p) for p in preds]
        for i, p in enumerate(preds):
            for j in p:
                succs[j].append(i)
        import heapq
        ready_t = [0.0] * n
        finish = [0.0] * n
        eng_free = {e: 0.0 for e in ENGINES}
        ready = {e: [] for e in ENGINES}
        for i in range(n):
            if npred[i] == 0:
                heapq.heappush(ready[ops[i][1]], i)
        order = []
        done = 0
        while done < n:
            best = None
            for e in ENGINES:
                cand = None
                for i in ready[e][:48]:
                    st = max(eng_free[e], ready_t[i])
                    if cand is None or (st, i) < cand[:2]:
                        cand = (st, i, e)
                if cand is not None and (best is None or cand[:2] < best[:2]):
                    best = cand
            st, i, e = best
            ready[e].remove(i)
            heapq.heapify(ready[e])
            kind, _, fn, reads, writes, cost, extra = ops[i]
            if kind == "dma":
                eng_free[e] = st + 150.0
            else:
                eng_free[e] = st + cost
            finish[i] = st + cost
            order.append(i)
            done += 1
            for j in succs[i]:
                ready_t[j] = max(ready_t[j], finish[i] + sync_lat)
                npred[j] -= 1
                if npred[j] == 0:
                    heapq.heappush(ready[ops[j][1]], j)
        for i in order:
            kind, eng, fn, reads, writes, cost, extra = ops[i]
            if kind == "op":
                self.op(eng, fn, reads, writes)
            else:
                self.dma(eng, extra[0], extra[1], reads, writes, extra[2], fn=fn)
        return max(finish) if n else 0.0

    def barrier(self):
        assert self.cap is None
        self.enabled = True
        cur = {e: self.cnt[e] for e in ENGINES}
        cur.update(self.dma_cnt)
        for e in ENGINES:
            waits = []
            for k, v in cur.items():
                if v > 0 and self.known[e].get(k, 0) < v:
                    if k == e and e == "tensor":
                        continue
                    self.known[e][k] = v
                    waits.append((k, v))
            self.ops[e].append((waits, None, None))
        self.bufs.clear()

    def emit(self, nc, st):
        keys = list(ENGINES) + list(self.dma_cnt.keys())
        sem = {k: st.enter_context(nc.semaphore(f"s_{k}")) for k in keys}
        block = st.enter_context(nc.Block())

        def mk(name):
            def body(e):
                for waits, fn, inc in self.ops[name]:
                    for k, v in waits:
                        e.wait_ge(sem[k], v)
                    if fn is not None:
                        ins = fn(e)
                        ins.then_inc(sem[inc[0]], inc[1])
            return body
        block.sync(mk("sync"))
        block.scalar(mk("scalar"))
        block.vector(mk("vector"))
        block.gpsimd(mk("gpsimd"))
        block.tensor(mk("tensor"))


class Arena:
    def __init__(self, tens, size):
        self.t = tens
        self.size = size
        self.off = 0

    def reset(self):
        self.off = 0

    def alloc(self, n):
        o = self.off
        self.off += n
        assert self.off <= self.size, (self.off, self.size)
        return self.t[:, o:o + n]


def v3(ap, a):
    return ap.rearrange("p (a b) -> p a b", a=a)


def bc(ap, shape):
    return ap.to_broadcast(list(shape))


def build_program(n_layers=L, dbg=None):
    nc = bass.Bass("TRN2", target_bir_lowering=False)
    P = Prog()
    P.add_pool("x", 8)
    P.add_pool("st", 6)
    P.add_pool("w", 6)
    P.add_pool("m", 4)
    P.add_pool("ix", 8)

    def din(name, shape):
        return nc.dram_tensor(name, list(shape), F32, kind="ExternalInput").ap()
    x_in = din("x", [S, D])
    mem_in = din("mem", [MEM, D])
    w_in = din("w_in", [L, D, 2048])
    w_out = din("w_out", [L, D, D])
    w_mem = din("w_mem", [L, D, 512])
    NWR = L * NE * 128 * 2
    wgr = din("wgr", [NWR, 2048])
    wur = din("wur", [NWR, 2048])
    wdr = din("wdr", [NWR, 2048])
    w_router = din("w_router", [D, NE])
    cst = din("cst", [128, 1024])
    biasT = din("biasT", [128, 2048])
    maskf = din("maskf", [128, 2048])
    bfm_in = din("bfm", [128, L * NFM])
    btm_in = din("btm", [L, TMW])
    lbl_in = din("lbl", [128, L * 2])
    normw_in = din("normw", [128, L])
    sink_in = din("sinks", [128, L * 4])
    ln_in = din("lnp", [L, 4, D])
    rb_in = din("rbias", [1, NE])
    out = nc.dram_tensor("out", [S, D], F32, kind="ExternalOutput").ap()
    xa = nc.dram_tensor("xa", [S, D], F32, kind="Internal").ap()
    xb = nc.dram_tensor("xb", [S, D], F32, kind="Internal").ap()
    xs_d = nc.dram_tensor("xs_d", [NSLOT, XW], F32, kind="Internal").ap()
    ys_d = nc.dram_tensor("ys_d", [NSLOT, D], F32, kind="Internal").ap()

    with ExitStack() as st:
        E = st.enter_context

        def sb(name, shape, dt=F32):
            return E(nc.sbuf_tensor(name, list(shape), dt))
        WBUF = sb("WBUF", [128, 28672], BF16)
        AF32 = Arena(sb("AF32", [128, 15360], F32), 15360)
        ABF = Arena(sb("ABF", [128, 20736], BF16), 20736)
        cst_sb = sb("cst_sb", [128, 1024])
        tri = cst_sb[:, 640:768]
        ones128 = cst_sb[:, 768:896]
        iota2p = cst_sb[:, 896:897]
        thr = cst_sb[:, 897:905]
        jiota = cst_sb[:, 905:905 + NJ]
        slot_i = sb("slot_i", [128, NB], I32)
        widx_i = sb("widx_i", [128, NJ * 8], I32)
        ident = cst_sb[:, 0:128]
        hgmask = cst_sb[:, 128:256]
        resetm = cst_sb[:, 256:384]
        onesbd = cst_sb[:, 384:512]
        cmask = cst_sb[:, 512:520]
        expB = sb("expB", [128, 2048])
        ones_bf = sb("ones_bf", [128, 64], BF16)
        bfm = sb("bfm_sb", [128, L * NFM])
        nbfm = sb("nbfm_sb", [128, L * NFM])
        btm_bc = sb("btm_bc", [128, TMW])
        lb = sb("lb_sb", [128, L * 2])
        oml = sb("oml_sb", [128, L * 2])
        noml = sb("noml_sb", [128, L * 2])
        lbE = sb("lbE_sb", [128, L * 2])
        lbtmp = sb("lbtmp", [128, 8])
        normw = sb("normw_sb", [128, L])
        sinkE = sb("sinkE_sb", [128, L * 4])
        lnp = sb("lnp_sb", [128, 2 * D])
        rb_bc = sb("rb_bc", [128, NE])
        wr_sb = sb("wr_sb", [128, 8 * NE])
        memT = sb("memT", [128, 8 * MEM], BF16)
        mkT = sb("mkT", [128, 2 * MEM], BF16)
        mv_sb = sb("mv_sb", [128, 2 * 256], BF16)
        Sst = sb("Sst", [128, 2 * 2 * 8 * 64])
        Sbf = sb("Sbf", [128, 2 * 2 * 8 * 128], BF16)
        rout = sb("rout", [128, 4 * NB * 4])
        runp = sb("runp", [128, 4])
        Cst = sb("Cst", [128, 2 * 3 * 64])
        Cbf = sb("Cbf", [128, 2 * 3 * 128], BF16)
        ps = [E(nc.psum_tensor(f"ps{b}", [128, 512], F32)) for b in range(8)]
        bank_rr = [0]

        def bank():
            b = bank_rr[0]
            bank_rr[0] = (b + 1) % 8
            return b, ps[b], ("ps", b)

        def fsz(ap):
            return float(ap.free_size())

        def ACT(out, in_, func, bias=0.0, scale=1.0, r=(), w=()):
            P.op("scalar", lambda e: e.activation(out=out, in_=in_, func=func, bias=bias, scale=scale), r, w, cost=220.0 + 0.85 * fsz(in_))

        def TT(eng, out, in0, in1, op, r=(), w=()):
            P.op(eng, lambda e: e.tensor_tensor(out=out, in0=in0, in1=in1, op=op), r, w, cost=(120.0 + 1.05 * fsz(out)) if eng == "vector" else (250.0 + 2.3 * fsz(out)))

        def TS(eng, out, in0, s1, s2, op0, op1=None, r=(), w=()):
            if op1 is None:
                P.op(eng, lambda e: e.tensor_scalar(out=out, in0=in0, scalar1=s1, scalar2=None, op0=op0), r, w, cost=(120.0 + 1.05 * fsz(out)) if eng == "vector" else (250.0 + 2.3 * fsz(out)))
            else:
                P.op(eng, lambda e: e.tensor_scalar(out=out, in0=in0, scalar1=s1, scalar2=s2, op0=op0, op1=op1), r, w, cost=(120.0 + 1.05 * fsz(out)) if eng == "vector" else (250.0 + 2.3 * fsz(out)))

        def STT(out, in0, scalar, in1, op0, op1, r=(), w=()):
            P.op("vector", lambda e: e.scalar_tensor_tensor(out=out, in0=in0, scalar=scalar, in1=in1, op0=op0, op1=op1), r, w, cost=120.0 + 1.05 * fsz(out))

        def RECIP(out, in_, r=(), w=()):
            P.op("vector", lambda e: e.reciprocal(out=out, in_=in_), r, w, cost=120.0 + 1.05 * fsz(out))

        def COPY(eng, out, in_, r=(), w=()):
            if eng == "scalar":
                P.op("scalar", lambda e: e.copy(out=out, in_=in_), r, w, cost=220.0 + 0.85 * fsz(out))
            else:
                P.op(eng, lambda e: e.tensor_copy(out=out, in_=in_), r, w, cost=(120.0 + 1.05 * fsz(out)) if eng == "vector" else (250.0 + 2.3 * fsz(out)))

        def MMG(mms, r=(), w=()):
            def fn(e):
                ins = None
                for (o, l_, r_, s0, s1, tp) in mms:
                    if tp is None:
                        ins = e.matmul(o, l_, r_, start=s0, stop=s1)
                    else:
                        ins = e.matmul(o, l_, r_, start=s0, stop=s1, tile_position=tp)
                return ins
            P.op("tensor", fn, r, w, cost=60.0 + sum(max(64.0, fsz(m_[2])) * 0.45 for m_ in mms))

        def TRG(trs, r=(), w=()):
            def fn(e):
                ins = None
                for (o, i_) in trs:
                    ins = e.transpose(o, i_, ident)
                return ins
            P.op("tensor", fn, r, w, cost=60.0 + 110.0 * len(trs))

        def MEMSET(eng, ap, val, w=()):
            P.op(eng, lambda e: e.memset(ap, val), (), w)

        P.dma("sync", cst_sb[:], cst, (), ["cst"], "m")
        P.dma("sync", expB[:], biasT, (), ["expB"], "m")
        mtmp = AF32.alloc(2048)
        P.dma("sync", mtmp, maskf, (), ["mtmp"], "m")
        P.dma("sync", bfm[:], bfm_in, (), ["bfm"], "m")
        P.dma("sync", lbE[:], lbl_in, (), ["lbE"], "m")
        P.dma("sync", normw[:], normw_in, (), ["normw"], "m")
        P.dma("sync", sinkE[:], sink_in, (), ["sinkE"], "m")
        P.dma("sync", rb_bc[:], rb_in.partition_broadcast(128), (), ["rb"], "m")
        P.dma("sync", v3(wr_sb[:], 8), w_router.rearrange("(k p) n -> p k n", p=128), (), ["wr"], "m")
        ztile = AF32.alloc(4 * XW)
        MEMSET("gpsimd", ztile, 0.0, ["ztile"])
        for j_ in range(NJ):
            P.dma("sync", xs_d[j_ * 512:(j_ + 1) * 512, :].rearrange("(a p) w -> p a w", p=128), v3(ztile, 4), ["ztile"], [("xsz", j_)], "st")
        MEMSET("vector", ones_bf[:], 1.0, ["ones"])
        MEMSET("vector", Sst[:], 0.0, ["Sst"])
        MEMSET("gpsimd", Sbf[:], 0.0, ["Sbf"])
        MEMSET("vector", Cst[:], 0.0, ["Cst"])
        MEMSET("gpsimd", Cbf[:], 0.0, ["Cbf"])
        ACT(expB[:], expB[:], AF.Exp, r=["expB"], w=["expB"])
        TT("vector", expB[:], expB[:], mtmp, ALU.mult, r=["expB", "mtmp"], w=["expB"])
        ACT(sinkE[:], sinkE[:], AF.Exp, r=["sinkE"], w=["sinkE"])
        TS("vector", nbfm[:], bfm[:], -1.0, None, ALU.mult, r=["bfm"], w=["nbfm"])
        ACT(lbE[:], lbE[:], AF.Exp, r=["lbE"], w=["lbE"])
        lbE3 = v3(lbE[:], L)
        tot = lbtmp[:, 0:2]
        rtot = lbtmp[:, 2:4]
        cum = lbtmp[:, 4:6]
        TT("vector", tot, lbE3[:, 0, :], lbE3[:, 1, :], ALU.add, r=["lbE"], w=["lbtmp"])
        for l_ in range(2, L):
            TT("vector", tot, tot, lbE3[:, l_, :], ALU.add, r=["lbE", "lbtmp"], w=["lbtmp"])
        RECIP(rtot, tot, r=["lbtmp"], w=["lbtmp"])
        lb3 = v3(lb[:], L)
        MEMSET("vector", lb3[:, 0, :], 0.0, ["lb"])
        for l_ in range(1, L):
            if l_ == 1:
                COPY("vector", cum, lbE3[:, 1, :], r=["lbE", "lbtmp"], w=["lbtmp"])
            else:
                TT("vector", cum, cum, lbE3[:, l_, :], ALU.add, r=["lbE", "lbtmp"], w=["lbtmp"])
            TT("vector", lb3[:, l_, :], cum, rtot, ALU.mult, r=["lbtmp", "lb"], w=["lb"])
        TS("vector", oml[:], lb[:], -1.0, 1.0, ALU.mult, ALU.add, r=["lb"], w=["oml"])
        TS("vector", noml[:], oml[:], -1.0, None, ALU.mult, r=["oml"], w=["noml"])
        for mc in range(2):
            mt = AF32.alloc(1024)
            P.dma("sync", mt, mem_in[mc * 128:(mc + 1) * 128, :], (), [("mt", mc)], "x")
            for hb in range(2):
                b, pb, pk = bank()
                TRG([(pb[:, q * 128:(q + 1) * 128], mt[:, (hb * 4 + q) * 128:(hb * 4 + q + 1) * 128]) for q in range(4)],
                    r=[("mt", mc), "cst"], w=[pk])
                dst = v3(memT[:], 8)[:, hb * 4:(hb + 1) * 4, mc * 128:(mc + 1) * 128]
                COPY("vector", dst, v3(pb[:], 4), r=[pk], w=["memT"])
        P.barrier()

        def layer_norm(zt, gsl, bsl, key, eng_g="gpsimd", eng_b="gpsimd", tmp=None):
            if tmp is None:
                tmp = AF32.alloc(18)
            stats, mvv, sm = tmp[:, 0:12], tmp[:, 12:14], tmp[:, 14:18]
            for h in range(2):
                P.op("vector", lambda e, h=h: e.bn_stats(out=stats[:, h * 6:(h + 1) * 6], in_=zt[:, h * 512:(h + 1) * 512]),
                     [key], [(key, "_st")])
            P.op("vector", lambda e: e.bn_aggr(out=mvv, in_=stats), [(key, "_st")], [(key, "_mv")])
            ACT(sm[:, 0:1], mvv[:, 1:2], AF.Ln, bias=LN_EPS, r=[(key, "_mv")], w=[(key, "_sm")])
            ACT(sm[:, 1:2], sm[:, 0:1], AF.Exp, scale=-0.5, r=[(key, "_sm")], w=[(key, "_sm")])
            TS("vector", sm[:, 2:3], mvv[:, 0:1], -1.0, sm[:, 1:2], ALU.mult, ALU.mult, r=[(key, "_mv"), (key, "_sm")], w=[(key, "_sm2")])
            ACT(zt, zt, AF.Identity, bias=sm[:, 2:3], scale=sm[:, 1:2], r=[key, (key, "_sm"), (key, "_sm2")], w=[key])
            TT(eng_g, zt, zt, gsl, ALU.mult, r=[key, "lnp"], w=[key])
            TT(eng_b, zt, zt, bsl, ALU.add, r=[key, "lnp"], w=[key])

        def load_ln(l, which):
            for j in range(2):
                P.dma("sync", lnp[:, j * D:(j + 1) * D], ln_in[l, 2 * which + j:2 * which + j + 1, :].partition_broadcast(128), (), ["lnp"], "m")


        def route_block(b, xtile, kxl, xT32b, ktl, R, rs, kr, ing3, posb3, base3, wq3, run, wr3):
            xT32_3 = v3(xT32b, 8)
            for hb in range(2):
                bk, pb, pk = bank()
                TRG([(pb[:, q * 128:(q + 1) * 128], xtile[:, (hb * 4 + q) * 128:(hb * 4 + q + 1) * 128]) for q in range(4)],
                    r=kxl + ["cst"], w=[pk])
                COPY("scalar" if hb == 0 else "vector", xT32_3[:, hb * 4:(hb + 1) * 4, :], v3(pb[:], 4), r=[pk], w=[ktl[hb]])
            bk, pb, pk = bank()
            MMG([(pb[:, 0:NE], xT32_3[:, k, :], wr3[:, k, :], k == 0, k == 7, None) for k in range(8)], r=ktl + ["wr"], w=[pk])
            ACT(R["en"], pb[:, 0:NE], AF.Exp, scale=-1.0, r=[pk], w=[kr])
            TS("vector", R["en"], R["en"], 1.0, None, ALU.add, r=[kr], w=[kr])
            RECIP(R["sc"], R["en"], r=[kr], w=[kr])
            TT("vector", R["sel"], R["sc"], rb_bc[:], ALU.add, r=[kr, "rb"], w=[kr])
            sel3 = v3(R["sel"], 4)
            m1, m2, gsum, gmax, wsum, rws = rs[:, 0:4], rs[:, 4:8], rs[:, 8:12], rs[:, 12:13], rs[:, 20:21], rs[:, 21:22]
            ing = ing3[:, b, :]
            P.op("vector", lambda e: e.tensor_reduce(out=m1, in_=sel3, axis=AX.X, op=ALU.max), [kr], [kr])
            TT("vector", v3(R["eq"], 4), sel3, bc(m1.unsqueeze(2), [128, 4, 4]), ALU.is_equal, r=[kr], w=[kr])
            STT(R["sel2"], R["eq"], -1.0e9, R["sel"], ALU.mult, ALU.add, r=[kr], w=[kr])
            P.op("vector", lambda e: e.tensor_reduce(out=m2, in_=v3(R["sel2"], 4), axis=AX.X, op=ALU.max), [kr], [kr])
            TT("vector", gsum, m1, m2, ALU.add, r=[kr], w=[kr])
            P.op("vector", lambda e: e.tensor_reduce(out=gmax, in_=gsum, axis=AX.X, op=ALU.max), [kr], [kr])
            TS("vector", ing, gsum, gmax, None, ALU.is_equal, r=[kr], w=[kr, ("ing", b)])
            TT("vector", v3(R["ge"], 4), sel3, bc(m2.unsqueeze(2), [128, 4, 4]), ALU.is_ge, r=[kr], w=[kr])
            TT("vector", v3(R["selm"], 4), v3(R["ge"], 4), bc(ing.unsqueeze(2), [128, 4, 4]), ALU.mult, r=[kr, ("ing", b)], w=[kr])
            TT("vector", R["wsel"], R["sc"], R["selm"], ALU.mult, r=[kr], w=[kr])
            P.op("vector", lambda e: e.tensor_reduce(out=wsum, in_=R["wsel"], axis=AX.X, op=ALU.add), [kr], [kr])
            RECIP(rws, wsum, r=[kr], w=[kr])
            TS("vector", R["wtb"], R["wsel"], rws, None, ALU.mult, r=[kr], w=[kr])
            P.op("vector", lambda e: e.tensor_reduce(out=wq3[:, b, :], in_=R["wtb"].rearrange("p (g q) -> p q g", g=4), axis=AX.X, op=ALU.add),
                 [kr], [("wq", b)])
            bk2, pb2, pk2 = bank()
            MMG([(pb2[:, 0:4], tri, ing, True, True, None), (pb2[:, 4:8], ones128, ing, True, True, None)], r=[("ing", b), "cst"], w=[pk2])
            COPY("vector", posb3[:, b, :], pb2[:, 0:4], r=[pk2], w=[("posb", b)])
            COPY("vector", base3[:, b, :], run, r=["run"], w=[("base", b)])
            TT("vector", run, run, pb2[:, 4:8], ALU.add, r=["run", pk2], w=["run"])

        prefetched = set()

        def load_A_weights(l):
            winv = v3(WBUF[:, 0:16384], 8)
            woutv = v3(WBUF[:, 16384:24576], 8)
            wmemv = v3(WBUF[:, 24576:28672], 8)
            for k2 in range(4):
                P.dma("gpsimd", winv[:, 2 * k2:2 * k2 + 2, :], w_in[l].rearrange("(k p) n -> p k n", p=128)[:, 2 * k2:2 * k2 + 2, :], (), [("W", k2)], "w")
            P.dma("gpsimd", wmemv, w_mem[l].rearrange("(k p) n -> p k n", p=128), (), [("W", 6)], "w")
            for k2 in range(2):
                P.dma("gpsimd", woutv[:, 4 * k2:4 * k2 + 4, :], w_out[l].rearrange("(k p) n -> p k n", p=128)[:, 4 * k2:4 * k2 + 4, :], (), [("W", 4 + k2)], "w")

        def phase_A(l, src, dst):
            AF32.reset()
            ABF.reset()
            winv = v3(WBUF[:, 0:16384], 8)
            woutv = v3(WBUF[:, 16384:24576], 8)
            wmemv = v3(WBUF[:, 24576:28672], 8)
            if l not in prefetched:
                load_A_weights(l)
            P.dma("sync", btm_bc[:], btm_in[l:l + 1, :].partition_broadcast(128), (), ["btm"], "m")
            load_ln(l, 0)
            WIN = [("W", i) for i in range(4)]
            WOUT = [("W", 4), ("W", 5)]
            memT3 = v3(memT[:], 8)
            mkT3 = v3(mkT[:], 2)
            for pr in range(2):
                b, pb, pk = bank()
                MMG([(pb[:, 0:256], wmemv[:, k, pr * 128:(pr + 1) * 128], memT3[:, k, :], k == 0, k == 7, None) for k in range(8)],
                    r=[("W", 6), "memT"], w=[pk])
                COPY("vector", mkT3[:, pr, :], pb[:, 0:256], r=[pk], w=["mkT"])
            mv3 = v3(mv_sb[:], 2)
            for mc in range(2):
                b, pb, pk = bank()
                MMG([(pb[:, 0:256], memT3[:, k, mc * 128:(mc + 1) * 128], wmemv[:, k, 256:512], k == 0, k == 7, None) for k in range(8)],
                    r=[("W", 6), "memT"], w=[pk])
                COPY("scalar", mv3[:, mc, :], pb[:, 0:256], r=[pk], w=["mv"])

            xt = [AF32.alloc(1024) for _ in range(2)]
            pfs = [AF32.alloc(NFM * 128) for _ in range(2)]
            denss = [AF32.alloc(512) for _ in range(2)]
            lnts = [AF32.alloc(18) for _ in range(2)]
            xT32r = AF32.alloc(1024)
            rtsA = [{nm: AF32.alloc(16) for nm in ["en", "sc", "sel", "eq", "sel2", "ge", "selm", "wsel", "wtb"]} for _ in range(2)]
            rssA = [AF32.alloc(32) for _ in range(2)]
            routv = rout[:].rearrange("p (a b c) -> p a b c", a=4, b=NB)
            ing3, posb3, base3, wq3 = routv[:, 0], routv[:, 1], routv[:, 2], routv[:, 3]
            wr3 = v3(wr_sb[:], 8)
            MEMSET("vector", runp[:], 0.0, ["run"])
            mrecs = [AF32.alloc(256) for _ in range(2)]
            hg = {}
            for nm in ["e1", "logf", "kk", "bcum", "eb", "enb", "eq", "qs", "kt32", "kh32", "osb", "lnms", "eg"]:
                hg[nm] = [AF32.alloc(256) for _ in range(2)]
            xT = [ABF.alloc(1024) for _ in range(2)]
            qTs = [ABF.alloc(512) for _ in range(2)]
            kT = [ABF.alloc(128) for _ in range(3)]
            swaV = [ABF.alloc(128) for _ in range(3)]
            hgVs = [ABF.alloc(256) for _ in range(2)]
            mqTs = [ABF.alloc(256) for _ in range(2)]
            Pts = [ABF.alloc(2048) for _ in range(2)]
            mixT = [ABF.alloc(1024) for _ in range(2)]
            PmTs = [ABF.alloc(1024) for _ in range(2)]
            qtTs = [ABF.alloc(256) for _ in range(2)]
            ktTs = [ABF.alloc(256) for _ in range(2)]
            khTs = [ABF.alloc(256) for _ in range(2)]
            qbds = [ABF.alloc(512) for _ in range(2)]
            for sl_ in range(2):
                MEMSET("gpsimd", qbds[sl_], 0.0, [(("hg", sl_), "qbd0"), (("hg", sl_), "qbd1")])
            Vblks = [ABF.alloc(2048) for _ in range(2)]
            scTs = [ABF.alloc(512) for _ in range(2)]
            Sst5 = Sst[:].rearrange("p (a b c d) -> p a b c d", a=2, b=2, c=8)
            Sbf5 = Sbf[:].rearrange("p (a b c d) -> p a b c d", a=2, b=2, c=8)
            Cst4 = Cst[:].rearrange("p (a b d) -> p a b d", a=2, b=3)
            Cbf4 = Cbf[:].rearrange("p (a b d) -> p a b d", a=2, b=3)
            bfm3 = v3(bfm[:], L)
            lb3_ = v3(lb[:], L)
            oml3 = v3(oml[:], L)
            noml3 = v3(noml[:], L)
            sinkE3 = v3(sinkE[:], L)
            expB4 = expB[:].rearrange("p (a b c) -> p a b c", a=2, b=2)

            def hgrn_block(i, sl, pf3, hgV, Vblk, scT, mixT3, km):
                par = i % 2
                H = {k_: v_[sl] for k_, v_ in hg.items()}
                qtT, ktT, khT, qbd = qtTs[sl], ktTs[sl], khTs[sl], qbds[sl]
                kp = ("hg", sl)
                kv = ("hgV", sl)
                Q2, F2, G2 = pf3[:, 5:7, :], pf3[:, 7:9, :], pf3[:, 9:11, :]
                rq, rf, rg = [("pf", sl, 4)], [("pf", sl, 4), ("pf", sl, 8)], [("pf", sl, 8)]
                A2 = lambda nm: v3(H[nm], 2)

                def sigm(dst, src3, rr, key):
                    ACT(A2(dst), src3, AF.Exp, scale=-1.0, r=rr, w=[key])
                    ACT(H[dst], H[dst], AF.Ln, bias=1.0, r=[key], w=[key])
                    ACT(H[dst], H[dst], AF.Exp, scale=-1.0, r=[key], w=[key])
                sigm("e1", F2, rf, (kp, "e1"))
                sigm("eq", Q2, rq, (kp, "eq"))
                sigm("eg", G2, rg, (kp, "eg"))
                for pr in range(2):
                    sl_ = slice(pr * 128, (pr + 1) * 128)
                    ACT(H["logf"][:, sl_], H["e1"][:, sl_], AF.Ln, bias=lb3_[:, l, pr:pr + 1], scale=oml3[:, l, pr:pr + 1], r=[(kp, "e1"), "lb", "oml"], w=[(kp, "logf", pr)])
                    TS("gpsimd", H["kk"][:, sl_], H["e1"][:, sl_], noml3[:, l, pr:pr + 1], oml3[:, l, pr:pr + 1], ALU.mult, ALU.add, r=[(kp, "e1"), "oml", "noml"], w=[(kp, "kk", pr)])
                    P.op("vector", lambda e, H=H, sl_=sl_: e.tensor_tensor_scan(out=H["bcum"][:, sl_], data0=resetm, data1=H["logf"][:, sl_], initial=0.0, op0=ALU.mult, op1=ALU.add),
                         [(kp, "logf", pr), "cst"], [(kp, "bcum", pr)], cost=400.0)
                BK = [(kp, "bcum", 0), (kp, "bcum", 1)]
                ACT(H["eb"], H["bcum"], AF.Exp, r=BK, w=[(kp, "eb")])
                ACT(H["enb"], H["bcum"], AF.Exp, scale=-1.0, r=BK, w=[(kp, "enb")])
                TT("gpsimd", A2("qs"), Q2, A2("eq"), ALU.mult, r=rq + [(kp, "eq")], w=[(kp, "qs")])
                TT("gpsimd", H["kt32"], H["kk"], H["enb"], ALU.mult, r=[(kp, "kk", 0), (kp, "kk", 1), (kp, "enb")], w=[(kp, "kt32")])
                STT(qtT, H["qs"], 0.125, H["eb"], ALU.mult, ALU.mult, r=[(kp, "qs"), (kp, "eb")], w=[(kp, "qtT")])
                COPY("gpsimd", ktT, H["kt32"], r=[(kp, "kt32")], w=[(kp, "ktT")])
                eb4 = H["eb"].rearrange("p (a c t) -> p a c t", a=2, c=8)
                TT("vector", v3(H["kh32"], 16), v3(H["kt32"], 16), bc(v3(H["eb"], 16)[:, :, 15:16], [128, 16, 16]), ALU.mult,
                   r=[(kp, "kt32"), (kp, "eb")], w=[(kp, "kh32")])
                qtT3 = v3(qtT, 2)
                qbd4 = qbd.rearrange("p (a r t) -> p a r t", a=2, r=2)
                COPY("gpsimd", qbd4[0:64, :, 0, :], qtT3[0:64, :, :], r=[(kp, "qtT")], w=[(kp, "qbd0")])
                COPY("gpsimd", qbd4[64:128, :, 1, :], qtT3[64:128, :, :], r=[(kp, "qtT")], w=[(kp, "qbd1")])
                bt, pbt, pkt = bank()
                TRG([(pbt[:, pr * 128:(pr + 1) * 128], H["kh32"][:, pr * 128:(pr + 1) * 128]) for pr in range(2)], r=[(kp, "kh32"), "cst"], w=[pkt])
                COPY("scalar", khT, pbt[:, 0:256], r=[pkt], w=[(kp, "khT")])
                Vb4 = Vblk.rearrange("p (h c v) -> p h c v", h=4, c=8)
                hgV3 = v3(hgV, 4)
                khT3 = v3(khT, 2)
                for h in range(4):
                    TT("gpsimd", Vb4[:, h, :, :], bc(hgV3[:, h, :].unsqueeze(1), [128, 8, 64]), bc(cmask.unsqueeze(2), [128, 8, 64]), ALU.mult,
                       r=[kv, "cst"], w=[("Vblk", sl, h)])
                pbus = []
                for pr in range(2):
                    bu, pbu, pku = bank()
                    pbus.append((pbu, pku))
                    mmu = []
                    for r_ in range(2):
                        h = 2 * pr + r_
                        mmu.append((pbu[r_ * 64:(r_ + 1) * 64, :], khT3[:, pr, r_ * 64:(r_ + 1) * 64],
                                    Vb4[:, h, :, :].rearrange("p c v -> p (c v)"), True, True, (0, r_ * 64)))
                    MMG(mmu, r=[(kp, "khT"), ("Vblk", sl, 2 * pr), ("Vblk", sl, 2 * pr + 1)], w=[pku])
                bsc, pbsc, pksc = bank()
                scT3 = v3(scT, 4)
                ktT3 = v3(ktT, 2)
                MMG([(pbsc[:, pr * 256:(pr + 1) * 256], ktT3[:, pr, :], qbd4[:, pr, :, :].rearrange("p r t -> p (r t)"), True, True, None) for pr in range(2)],
                    r=[(kp, "ktT"), (kp, "qbd0"), (kp, "qbd1")], w=[pksc])
                TT("vector", scT3, v3(pbsc[:, :], 4), bc(hgmask.unsqueeze(1), [128, 4, 128]), ALU.mult, r=[pksc, "cst"], w=[("scT", sl)])
                cin, cout = i % 3, (i + 1) % 3
                for pr in range(2):
                    pbu, pku = pbus[pr]
                    pu3 = v3(pbu[:], 8)
                    for c in range(8):
                        s_prev = Cst4[:, pr, cin, :] if c == 0 else Sst5[:, pr, par, c, :]
                        s_out = Cst4[:, pr, cout, :] if c == 7 else Sst5[:, pr, par, c + 1, :]
                        STT(s_out, s_prev, eb4[:, pr, c, 15:16], pu3[:, c, :], ALU.mult, ALU.add,
                            r=[pku, (kp, "eb"), ("Sst", pr, par), ("Cst", pr, cin)], w=[("Sst", pr, par)] + ([("Cst", pr, cout)] if c == 7 else []))
                    COPY("scalar", Sbf5[0:64, pr, par, 1:8, 0:64], Sst5[0:64, pr, par, 1:8, :], r=[("Sst", pr, par)], w=[("Sbf", pr, par, 0)])
                    COPY("scalar", Sbf5[64:128, pr, par, 1:8, 64:128], Sst5[64:128, pr, par, 1:8, :], r=[("Sst", pr, par)], w=[("Sbf", pr, par, 1)])
                    COPY("gpsimd", Cbf4[0:64, pr, cout, 0:64], Cst4[0:64, pr, cout, :], r=[("Cst", pr, cout)], w=[("Cbf", pr, cout, 0)])
                    COPY("gpsimd", Cbf4[64:128, pr, cout, 64:128], Cst4[64:128, pr, cout, :], r=[("Cst", pr, cout)], w=[("Cbf", pr, cout, 1)])
                bo, pbo, pko = bank()
                mmo = []
                for pr in range(2):
                    for r_ in range(2):
                        h = 2 * pr + r_
                        mmo.append((pbo[r_ * 64:(r_ + 1) * 64, pr * 128:(pr + 1) * 128], hgV3[:, h, :], scT3[:, h, :], True, False, (0, r_ * 64)))
                    for c in range(8):
                        sb_prev = Cbf4[:, pr, cin, :] if c == 0 else Sbf5[:, pr, par, c, :]
                        mmo.append((pbo[:, pr * 128 + c * 16:pr * 128 + (c + 1) * 16], sb_prev, qtT3[:, pr, c * 16:(c + 1) * 16], False, c == 7, None))
                rs_ = [kv, ("scT", sl), (kp, "qtT")]
                for pr in range(2):
                    rs_ += [("Sbf", pr, par, 0), ("Sbf", pr, par, 1), ("Cbf", pr, cin, 0), ("Cbf", pr, cin, 1)]
                MMG(mmo, r=rs_, w=[pko])
                COPY("scalar", H["osb"], pbo[:, 0:256], r=[pko], w=[(kp, "osb")])
                ACT(H["kh32"], pbo[:, 0:256], AF.Square, r=[pko], w=[(kp, "kh32")])
                bm, pbm, pkm = bank()
                MMG([(pbm[:, 0:256], onesbd, H["kh32"], True, True, None)], r=[(kp, "kh32"), "cst"], w=[pkm])
                TT("gpsimd", A2("eg"), G2, A2("eg"), ALU.mult, r=rg + [(kp, "eg")], w=[(kp, "eg")])
                ACT(H["lnms"], pbm[:, 0:256], AF.Ln, bias=RMS_EPS, r=[pkm], w=[(kp, "lnms")])
                ACT(H["lnms"], H["lnms"], AF.Exp, scale=-0.5, r=[(kp, "lnms")], w=[(kp, "lnms")])
                STT(H["osb"], H["osb"], normw[:, l:l + 1], H["lnms"], ALU.mult, ALU.mult, r=[(kp, "osb"), (kp, "lnms"), "normw"], w=[(kp, "osb")])
                TT("gpsimd", mixT3[:, 4:6, :], A2("osb"), A2("eg"), ALU.mult, r=[(kp, "osb"), (kp, "eg")], w=[(km, 1, 0), (km, 1, 1)])

            def blockgen(i):
                sl = i % 2
                s3 = i % 3
                p3 = (i - 1) % 3
                kx = ("xt", sl)
                pf3 = v3(pfs[sl], NFM)
                qT, hgV, mqT, Pt, PmT = qTs[sl], hgVs[sl], mqTs[sl], Pts[sl], PmTs[sl]
                dens, mrec = denss[sl], mrecs[sl]
                rec = dens
                P.dma("sync", xt[sl], src[0][i * 128:(i + 1) * 128, :], [(src[1], i)], [kx], "x")
                xT3 = v3(xT[sl], 8)
                for hb in range(2):
                    b, pb, pk = bank()
                    TRG([(pb[:, q * 128:(q + 1) * 128], xt[sl][:, (hb * 4 + q) * 128:(hb * 4 + q + 1) * 128]) for q in range(4)],
                        r=[kx, "cst"], w=[pk])
                    COPY("scalar", xT3[:, hb * 4:(hb + 1) * 4, :], v3(pb[:], 4), r=[pk], w=[("xT", sl, hb)])
                yield
                XTK = [("xT", sl, 0), ("xT", sl, 1)]
                for c0 in range(0, NFM, 4):
                    nch = min(4, NFM - c0)
                    b, pb, pk = bank()
                    mms = []
                    for c in range(c0, c0 + nch):
                        for k in range(8):
                            mms.append((pb[:, (c - c0) * 128:(c - c0 + 1) * 128], winv[:, k, c * 128:(c + 1) * 128], xT3[:, k, :], k == 0, k == 7, None))
                    MMG(mms, r=WIN + XTK, w=[pk])
                    TT("vector", pf3[:, c0:c0 + nch, :], v3(pb[:, 0:nch * 128], nch),
                       bc(bfm3[:, l, c0:c0 + nch].unsqueeze(2), [128, nch, 128]), ALU.add, r=[pk, "bfm"], w=[("pf", sl, c0)])
                    yield
                b, pb, pk = bank()
                MMG([(pb[:, 0:TMW], xT3[:, k, :], winv[:, k, NFM * 128:2048], k == 0, k == 7, None) for k in range(8)], r=WIN + XTK, w=[pk])
                TT("vector", swaV[s3], pb[:, 0:128], btm_bc[:, 0:128], ALU.add, r=[pk, "btm"], w=[("swaV", s3)])
                TT("vector", hgV, pb[:, 128:384], btm_bc[:, 128:384], ALU.add, r=[pk, "btm"], w=[("hgV", sl)])
                COPY("scalar", v3(qT, 4), pf3[:, 0:4, :], r=[("pf", sl, 0)], w=[("qT", sl)])
                COPY("gpsimd", kT[s3], pf3[:, 4, :], r=[("pf", sl, 4)], w=[("kT", s3)])
                COPY("gpsimd", v3(mqT, 2), pf3[:, 11:13, :], r=[("pf", sl, 8), ("pf", sl, 12)], w=[("mqT", sl)])
                yield
                qT3 = v3(qT, 4)
                Pt4 = Pt.rearrange("p (a b c) -> p a b c", a=2, b=2)
                pvs = [1] if i == 0 else [0, 1]
                for g in range(2):
                    for pv in pvs:
                        ksl = s3 if pv == 1 else p3
                        b, pb, pk = bank()
                        MMG([(pb[:, :], kT[ksl][g * 64:(g + 1) * 64, :], qT3[g * 64:(g + 1) * 64, :, :], True, True, None)],
                            r=[("kT", ksl), ("qT", sl)], w=[pk])
                        ACT(Pt4[:, pv, g, :], pb[:, :], AF.Exp, scale=0.125, r=[pk], w=[("Pt", sl, pv, g)])
                        TT("vector" if g == 0 else "gpsimd", Pt4[:, pv, g, :], Pt4[:, pv, g, :], expB4[:, pv, g, :], ALU.mult,
                           r=[("Pt", sl, pv, g), "expB"], w=[("Pt", sl, pv, g)])
                        yield
                bo, pbo, pko = bank()
                bd, pbd, pkd = bank()
                mmo, mmd = [], []
                for g in range(2):
                    for n_, pv in enumerate(pvs):
                        ksl = s3 if pv == 1 else p3
                        mmo.append((pbo[g * 64:(g + 1) * 64, :], swaV[ksl][:, g * 64:(g + 1) * 64], Pt4[:, pv, g, :], n_ == 0, n_ == len(pvs) - 1, (0, g * 64)))
                        mmd.append((pbd[g * 64:(g + 1) * 64, :], ones_bf[:, :], Pt4[:, pv, g, :], n_ == 0, n_ == len(pvs) - 1, (0, g * 64)))
                rr = [("Pt", sl, pv, g) for pv in pvs for g in range(2)] + [("swaV", s3), ("swaV", p3), "ones"]
                MMG(mmo, r=rr, w=[pko])
                MMG(mmd, r=rr, w=[pkd])
                yield
                TT("vector", v3(dens, 4), v3(pbd[:], 4), bc(sinkE3[:, l, :].unsqueeze(2), [128, 4, 128]), ALU.add, r=[pkd, "sinkE"], w=[("dens", sl)])
                RECIP(rec, dens, r=[("dens", sl)], w=[("dens", sl)])
                mixT3 = v3(mixT[sl], 8)
                km = ("mixT", sl)
                TT("vector", mixT3[:, 0:4, :], v3(pbo[:], 4), v3(rec, 4), ALU.mult, r=[pko, ("dens", sl)], w=[(km, 0)])
                yield
                mqT3 = v3(mqT, 2)
                PmT4 = PmT.rearrange("p (r a b c) -> p r a b c", r=2, a=2, b=2)
                for r_ in range(2):
                    b, pb, pk = bank()
                    mms = []
                    for mc in range(2):
                        for pr in range(2):
                            mms.append((pb[:, (mc * 2 + pr) * 128:(mc * 2 + pr + 1) * 128], mkT3[r_ * 64:(r_ + 1) * 64, pr, mc * 128:(mc + 1) * 128],
                                        mqT3[r_ * 64:(r_ + 1) * 64, pr, :], True, True, None))
                    MMG(mms, r=["mkT", ("mqT", sl)], w=[pk])
                    ACT(PmT4[:, r_].rearrange("p a b c -> p (a b c)"), pb[:, :], AF.Exp, scale=0.125, r=[pk], w=[("PmT", sl, r_)])
                    yield
                bo, pbo, pko = bank()
                mmo = []
                for kind in range(2):
                    for h in range(4):
                        pr, r_ = h // 2, h % 2
                        for mc in range(2):
                            lhs = mv3[:, mc, h * 64:(h + 1) * 64] if kind == 0 else ones_bf[:, :]
                            mmo.append((pbo[r_ * 64:(r_ + 1) * 64, kind * 256 + pr * 128:kind * 256 + (pr + 1) * 128], lhs,
                                        PmT4[:, r_, mc, pr, :], mc == 0, mc == 1, (0, r_ * 64)))
                MMG(mmo, r=[("PmT", sl, 0), ("PmT", sl, 1), "mv", "ones"], w=[pko])
                RECIP(mrec, pbo[:, 256:512], r=[pko], w=[("mrec", sl)])
                TT("vector", mixT3[:, 6:8, :], v3(pbo[:, 0:256], 2), v3(mrec, 2), ALU.mult, r=[pko, ("mrec", sl)], w=[(km, 2)])
                yield
                hgrn_block(i, sl, pf3, hgV, Vblks[sl], scTs[sl], mixT3, km)
                yield
                KM = [(km, 0), (km, 1, 0), (km, 1, 1), (km, 2)]
                for hf in range(2):
                    b, pb, pk = bank()
                    MMG([(pb[:, :], mixT3[:, m, :], woutv[:, m, hf * 512:(hf + 1) * 512], m == 0, m == 7, None) for m in range(8)],
                        r=KM + WOUT, w=[pk])
                    STT(xt[sl][:, hf * 512:(hf + 1) * 512], xt[sl][:, hf * 512:(hf + 1) * 512], ALPHA, pb[:, :], ALU.mult, ALU.add, r=[pk, kx], w=[kx])
                    yield
                layer_norm(xt[sl], lnp[:, 0:D], lnp[:, D:2 * D], kx, tmp=lnts[sl])
                P.dma("sync", dst[0][i * 128:(i + 1) * 128, :], xt[sl], [kx], [(dst[1], i)], "st")
                yield
                if not (dbg and dbg.get("phases") == "A"):
                    route_block(i, xt[sl], [kx], xT32r, [("xT32", 0), ("xT32", 1)], rtsA[sl], rssA[sl], ("rt", sl), ing3, posb3, base3, wq3, runp[:], wr3)

            nblk = dbg.get("nb", NB) if dbg else NB
            depth = dbg.get("depth", 2) if dbg else 2
            use_ls = (dbg.get("ls", 1) if dbg else 1)
            if use_ls:
                P.begin_capture()
            active, nxt = [], 0
            while nxt < nblk or active:
                if nxt < nblk and len(active) < depth:
                    active.append(blockgen(nxt))
                    nxt += 1
                for g_ in list(active):
                    try:
                        next(g_)
                    except StopIteration:
                        active.remove(g_)
            if use_ls:
                P.end_capture()

        def phase_B(l, src, dst):
            AF32.reset()
            ABF.reset()
            load_ln(l, 1)
            xt = [AF32.alloc(1024) for _ in range(2)]
            xT32 = AF32.alloc(1024)
            yacc = AF32.alloc(NBS * 1024)
            sg32 = [AF32.alloc(512) for _ in range(2)]
            wt_all = AF32.alloc(NBS * NE)
            rt = {nm: AF32.alloc(16) for nm in ["en", "sc", "sel", "eq", "sel2", "ge", "selm", "wsel"]}
            rs = AF32.alloc(32)
            xTm = ABF.alloc(8 * TSUP)
            hT = [ABF.alloc(2048) for _ in range(2)]
            xTm3 = v3(xTm, 8)
            yacc3 = v3(yacc, NBS)
            wt3 = v3(wt_all, NBS)
            wr3 = v3(wr_sb[:], 8)
            WG = lambda s_: v3(WBUF[:, (s_ * 3 + 0) * 4096:(s_ * 3 + 1) * 4096], 8)
            WU = lambda s_: v3(WBUF[:, (s_ * 3 + 1) * 4096:(s_ * 3 + 2) * 4096], 8)
            WD = lambda s_: v3(WBUF[:, (s_ * 3 + 2) * 4096:(s_ * 3 + 3) * 4096], 4)

            def load_expert(e):
                s_ = e % 2
                raise NotImplementedError("dense MoE path retired")

            for T in range(dbg.get("nT", S // TSUP) if dbg else S // TSUP):
                load_expert(0)
                P.enabled = (dbg is None) or (int(1) in dbg.get('bstages', range(10)))
                for blk in range(NBS):
                    i = T * NBS + blk
                    sl = blk % 2
                    kx = ("xt", sl)
                    P.enabled = (dbg is None) or (1 in dbg.get('bstages', range(10)))
                    P.dma("sync", xt[sl], src[0][i * 128:(i + 1) * 128, :], [(src[1], i)], [kx], "x")
                    xT32_3 = v3(xT32, 8)
                    for hb in range(2):
                        b, pb, pk = bank()
                        TRG([(pb[:, q * 128:(q + 1) * 128], xt[sl][:, (hb * 4 + q) * 128:(hb * 4 + q + 1) * 128]) for q in range(4)],
                            r=[kx, "cst"], w=[pk])
                        COPY("scalar", xT32_3[:, hb * 4:(hb + 1) * 4, :], v3(pb[:], 4), r=[pk], w=[("xT32", hb)])
                        COPY("vector", xTm3[:, hb * 4:(hb + 1) * 4, blk * 128:(blk + 1) * 128], v3(pb[:], 4), r=[pk], w=[("xTm", blk)])
                    b, pb, pk = bank()
                    if dbg and dbg.get("b1cut", 9) < 1:
                        P.enabled = False
                    MMG([(pb[:, 0:NE], xT32_3[:, k, :], wr3[:, k, :], k == 0, k == 7, None) for k in range(8)], r=[("xT32", 0), ("xT32", 1), "wr"], w=[pk])
                    R = rt
                    kr = "rt"
                    ACT(R["en"], pb[:, 0:NE], AF.Exp, scale=-1.0, r=[pk], w=[kr])
                    if dbg and dbg.get("b1cut", 9) < 2:
                        P.enabled = False
                    TS("vector", R["en"], R["en"], 1.0, None, ALU.add, r=[kr], w=[kr])
                    RECIP(R["sc"], R["en"], r=[kr], w=[kr])
                    TT("vector", R["sel"], R["sc"], rb_bc[:], ALU.add, r=[kr, "rb"], w=[kr])
                    sel3 = v3(R["sel"], 4)
                    m1, m2, gsum, gmax, ing, wsum, rws = rs[:, 0:4], rs[:, 4:8], rs[:, 8:12], rs[:, 12:13], rs[:, 16:20], rs[:, 20:21], rs[:, 21:22]
                    P.op("vector", lambda e, sel3=sel3, m1=m1: e.tensor_reduce(out=m1, in_=sel3, axis=AX.X, op=ALU.max), [kr], [kr])
                    TT("vector", v3(R["eq"], 4), sel3, bc(m1.unsqueeze(2), [128, 4, 4]), ALU.is_equal, r=[kr], w=[kr])
                    STT(R["sel2"], R["eq"], -1.0e9, R["sel"], ALU.mult, ALU.add, r=[kr], w=[kr])
                    P.op("vector", lambda e, R=R, m2=m2: e.tensor_reduce(out=m2, in_=v3(R["sel2"], 4), axis=AX.X, op=ALU.max), [kr], [kr])
                    TT("vector", gsum, m1, m2, ALU.add, r=[kr], w=[kr])
                    P.op("vector", lambda e, gsum=gsum, gmax=gmax: e.tensor_reduce(out=gmax, in_=gsum, axis=AX.X, op=ALU.max), [kr], [kr])
                    TS("vector", ing, gsum, gmax, None, ALU.is_equal, r=[kr], w=[kr])
                    TT("vector", v3(R["ge"], 4), sel3, bc(m2.unsqueeze(2), [128, 4, 4]), ALU.is_ge, r=[kr], w=[kr])
                    TT("vector", v3(R["selm"], 4), v3(R["ge"], 4), bc(ing.unsqueeze(2), [128, 4, 4]), ALU.mult, r=[kr], w=[kr])
                    TT("vector", R["wsel"], R["sc"], R["selm"], ALU.mult, r=[kr], w=[kr])
                    P.op("vector", lambda e, R=R, wsum=wsum: e.tensor_reduce(out=wsum, in_=R["wsel"], axis=AX.X, op=ALU.add), [kr], [kr])
                    RECIP(rws, wsum, r=[kr], w=[kr])
                    TS("vector", wt3[:, blk, :], R["wsel"], rws, None, ALU.mult, r=[kr], w=[("wt", blk)])
                P.enabled = (dbg is None) or (int(2) in dbg.get('bstages', range(10)))
                pend = None

                def down(e, sub, hs):
                    s_ = e % 2
                    hT3 = v3(hT[hs], 4)
                    for b4 in range(4):
                        blk = sub * 4 + b4
                        for hf in range(2):
                            b, pb, pk = bank()
                            MMG([(pb[:, :], hT3[:, jc, b4 * 128:(b4 + 1) * 128], WD(s_)[:, jc, hf * 512:(hf + 1) * 512], jc == 0, jc == 3, None) for jc in range(4)],
                                r=[("hT", hs, jc) for jc in range(4)] + [("W", s_ * 3 + 2)], w=[pk])
                            ysl = yacc3[:, blk, hf * 512:(hf + 1) * 512]
                            if e == 0:
                                TS("vector", ysl, pb[:, :], wt3[:, blk, e:e + 1], None, ALU.mult, r=[pk, ("wt", blk)], w=[("yacc", blk, hf)])
                            else:
                                STT(ysl, pb[:, :], wt3[:, blk, e:e + 1], ysl, ALU.mult, ALU.add, r=[pk, ("wt", blk), ("yacc", blk, hf)], w=[("yacc", blk, hf)])
                n_unit = 0
                for e in range(NE):
                    s_ = e % 2
                    for sub in range(TSUP // 512):
                        hs = n_unit % 2
                        hT3 = v3(hT[hs], 4)
                        for jc in range(4):
                            bg, pbg, pkg = bank()
                            bu, pbu, pku = bank()
                            MMG([(pbg[:, :], WG(s_)[:, k, jc * 128:(jc + 1) * 128], xTm3[:, k, sub * 512:(sub + 1) * 512], k == 0, k == 7, None) for k in range(8)],
                                r=[("W", s_ * 3 + 0)] + [("xTm", sub * 4 + q) for q in range(4)], w=[pkg])
                            MMG([(pbu[:, :], WU(s_)[:, k, jc * 128:(jc + 1) * 128], xTm3[:, k, sub * 512:(sub + 1) * 512], k == 0, k == 7, None) for k in range(8)],
                                r=[("W", s_ * 3 + 1)] + [("xTm", sub * 4 + q) for q in range(4)], w=[pku])
                            sgb = sg32[jc % 2]
                            ACT(sgb, pbg[:, :], AF.Silu, r=[pkg], w=[("sg", jc % 2)])
                            TT("vector", hT3[:, jc, :], sgb, pbu[:, :], ALU.mult, r=[("sg", jc % 2), pku], w=[("hT", hs, jc)])
                        if pend is not None:
                            down(*pend)
                        if sub == 0 and e + 1 < NE:
                            load_expert(e + 1)
                        pend = (e, sub, hs)
                        n_unit += 1
                down(*pend)
                P.enabled = (dbg is None) or (int(3) in dbg.get('bstages', range(10)))
                for blk in range(NBS):
                    i = T * NBS + blk
                    sl = blk % 2
                    kx = ("xt", sl)
                    P.dma("sync", xt[sl], src[0][i * 128:(i + 1) * 128, :], [(src[1], i)], [kx], "x")
                    for hf in range(2):
                        STT(xt[sl][:, hf * 512:(hf + 1) * 512], xt[sl][:, hf * 512:(hf + 1) * 512], ALPHA, yacc3[:, blk, hf * 512:(hf + 1) * 512], ALU.mult, ALU.add,
                            r=[kx, ("yacc", blk, hf)], w=[kx])
                    layer_norm(xt[sl], lnp[:, 0:D], lnp[:, D:2 * D], kx)
                    P.dma("sync", dst[0][i * 128:(i + 1) * 128, :], xt[sl], [kx], [(dst[1], i)], "st")

        def phase_B2(l, src, dst):
            AF32.reset()
            ABF.reset()
            load_ln(l, 1)
            nbB = dbg.get("nb", NB) if dbg else NB
            nj = (nbB * 128 + 4 * 511) // 512
            xr = [AF32.alloc(XW) for _ in range(2)]
            xT32 = AF32.alloc(1024)
            routv = rout[:].rearrange("p (a b c) -> p a b c", a=4, b=NB)
            ing3, posb3, base3, wq3 = routv[:, 0], routv[:, 1], routv[:, 2], routv[:, 3]
            run = runp[:]
            slot_f = AF32.alloc(NB)
            rts = [{nm: AF32.alloc(16) for nm in ["en", "sc", "sel", "eq", "sel2", "ge", "selm", "wsel", "wtb"]} for _ in range(4)]
            rss = [AF32.alloc(32) for _ in range(4)]
            gl = AF32.alloc(64)
            tmp4 = [AF32.alloc(4) for _ in range(2)]
            widx_f = AF32.alloc(NJ * 8)
            cmp1 = AF32.alloc(32)
            cmp2 = AF32.alloc(NJ * 4)
            xsr = [AF32.alloc(XW) for _ in range(4)]
            yacc = AF32.alloc(4096)
            sg32 = [AF32.alloc(512) for _ in range(2)]
            wqs = [AF32.alloc(16) for _ in range(2)]
            yg = [xsr[0][:, 0:1024], xsr[1][:, 0:1024]]
            xTm = ABF.alloc(8 * 512)
            hT = [ABF.alloc(2048) for _ in range(2)]
            xTm3 = v3(xTm, 8)
            yacc3 = v3(yacc, 4)
            wr3 = v3(wr_sb[:], 8)
            for sl_ in range(2):
                MEMSET("gpsimd", xr[sl_][:, 1024:XW], 0.0, [("xrpad", sl_)])
                MEMSET("gpsimd", xsr[sl_][:, 1024:XW], 0.0, [("xsrpad", sl_)])

            WG = lambda s_: WBUF[:, (s_ * 3 + 0) * 4096:(s_ * 3 + 1) * 4096]
            WU = lambda s_: WBUF[:, (s_ * 3 + 1) * 4096:(s_ * 3 + 2) * 4096]
            WD = lambda s_: WBUF[:, (s_ * 3 + 2) * 4096:(s_ * 3 + 3) * 4096]

            def load_w(j, q, s_):
                for m_, (dst_, srcw) in enumerate([(WG(s_), wgr), (WU(s_), wur), (WD(s_), wdr)]):
                    for hh in range(2):
                        col = (j * 4 + q) * 2 + hh
                        P.dma("gpsimd", None, None, ["widx"], [("W", s_ * 3 + m_, hh)], "w",
                              fn=(lambda e, dst_=dst_, srcw=srcw, hh=hh, col=col: e.indirect_dma_start(
                                  out=dst_[:, hh * 2048:(hh + 1) * 2048], out_offset=None, in_=srcw,
                                  in_offset=bass.IndirectOffsetOnAxis(ap=widx_i[:, col:col + 1], axis=0))))

            use_ls = (dbg.get("ls", 1) if dbg else 1)
            if use_ls:
                P.begin_capture()
            nbk, incl, pst, gidf, tg = gl[:, 0:4], gl[:, 4:8], gl[:, 8:12], gl[:, 16:16 + NJ], gl[:, 32:32 + NJ]
            KG = "glob"
            TT("vector", v3(cmp1, 4), bc(run.unsqueeze(2), [128, 4, 8]), bc(thr.unsqueeze(1), [128, 4, 8]), ALU.is_gt, r=["run", "cst"], w=[KG])
            P.op("vector", lambda e: e.tensor_reduce(out=nbk, in_=v3(cmp1, 4), axis=AX.X, op=ALU.add), [KG], [KG])
            COPY("vector", incl[:, 0:1], nbk[:, 0:1], r=[KG], w=[KG])
            for g in range(1, 4):
                TT("vector", incl[:, g:g + 1], incl[:, g - 1:g], nbk[:, g:g + 1], ALU.add, r=[KG], w=[KG])
            TT("vector", pst, incl, nbk, ALU.subtract, r=[KG], w=[KG])
            TS("vector", pst, pst, 512.0, None, ALU.mult, r=[KG], w=[KG])
            TT("vector", v3(cmp2, NJ), bc(incl.unsqueeze(1), [128, NJ, 4]), bc(jiota.unsqueeze(2), [128, NJ, 4]), ALU.is_le, r=[KG, "cst"], w=[KG])
            P.op("vector", lambda e: e.tensor_reduce(out=gidf, in_=v3(cmp2, NJ), axis=AX.X, op=ALU.add), [KG], [KG])
            TS("vector", gidf, gidf, 3.0, None, ALU.min, r=[KG], w=[KG])
            TS("vector", tg, gidf, 1024.0, iota2p, ALU.mult, ALU.add, r=[KG, "cst"], w=[KG])
            wf4 = widx_f.rearrange("p (j q h) -> p j q h", j=NJ, q=4)
            for q in range(4):
                for hh in range(2):
                    TS("vector", wf4[:, :, q, hh], tg, float((l * NE + q) * 256 + hh), None, ALU.add, r=[KG], w=[KG])
            COPY("vector", widx_i[:], widx_f, r=[KG], w=["widx"])
            load_w(0, 0, 0)
            for b in range(nbB):
                t4 = tmp4[b % 2]
                kt = ("tmp4", b % 2)
                TT("vector", t4, base3[:, b, :], posb3[:, b, :], ALU.add, r=[("base", b), ("posb", b)], w=[kt])
                TT("vector", t4, t4, pst, ALU.add, r=[kt, KG], w=[kt])
                TT("vector", t4, t4, ing3[:, b, :], ALU.mult, r=[kt, ("ing", b)], w=[kt])
                P.op("vector", lambda e, t4=t4, b=b: e.tensor_reduce(out=slot_f[:, b:b + 1], in_=t4, axis=AX.X, op=ALU.add), [kt], [("slotf", b)])
            COPY("vector", slot_i[:, 0:nbB], slot_f[:, 0:nbB], r=[("slotf", b) for b in range(nbB)], w=["slot_i"])
            p2x = [(xr[0], ("xr", 0), ("xrpad", 0)), (xr[1], ("xr", 1), ("xrpad", 1)), (xsr[0], ("xsr", 0), ("xsrpad", 0)), (xsr[1], ("xsr", 1), ("xsrpad", 1))]
            for b in range(nbB):
                row, kx, kpad = p2x[b % 4]
                P.dma("sync", row[:, 0:1024], src[0][b * 128:(b + 1) * 128, :], [(src[1], b)], [kx], "x")
                COPY("gpsimd", row[:, 1024:1028], wq3[:, b, :], r=[("wq", b)], w=[kpad])
                P.dma("gpsimd", None, None, [kx, kpad, "slot_i"] + [("xsz", j_) for j_ in range(NJ)], [("xs", b)], "ix",
                      fn=(lambda e, row=row, b=b: e.indirect_dma_start(out=xs_d, out_offset=bass.IndirectOffsetOnAxis(ap=slot_i[:, b:b + 1], axis=0),
                                                                    in_=row[:, :], in_offset=None)))
            XSK = [("xs", b) for b in range(nbB)]
            if use_ls:
                P.end_capture()

            def down(j, q, s_, hs):
                hT3 = v3(hT[hs], 4)
                wd3 = v3(WD(s_), 4)
                wq_v = v3(wqs[j % 2], 4)
                for b4 in range(4):
                    for hf in range(2):
                        bk, pb, pk = bank()
                        MMG([(pb[:, :], hT3[:, jc, b4 * 128:(b4 + 1) * 128], wd3[:, jc, hf * 512:(hf + 1) * 512], jc == 0, jc == 3, None) for jc in range(4)],
                            r=[("hT", hs, jc) for jc in range(4)] + [("W", s_ * 3 + 2, 0), ("W", s_ * 3 + 2, 1)], w=[pk])
                        ysl = yacc3[:, b4, hf * 512:(hf + 1) * 512]
                        if q == 0:
                            TS("vector", ysl, pb[:, :], wq_v[:, b4, q:q + 1], None, ALU.mult, r=[pk, ("wqs", j % 2, b4)], w=[("yacc", b4, hf)])
                        else:
                            STT(ysl, pb[:, :], wq_v[:, b4, q:q + 1], ysl, ALU.mult, ALU.add, r=[pk, ("wqs", j % 2, b4), ("yacc", b4, hf)], w=[("yacc", b4, hf)])
                if q == 3:
                    P.dma("sync", ys_d[j * 512:(j + 1) * 512, :].rearrange("(a p) d -> p a d", p=128), yacc3,
                          [("yacc", b4, hf) for b4 in range(4) for hf in range(2)], [("ys", j)], "st")

            pend = None
            n_unit = 0
            for j in range(nj):
                for b4 in range(4):
                    P.dma("sync", xsr[b4], xs_d[j * 512 + b4 * 128:j * 512 + (b4 + 1) * 128, :], XSK + [("xsz", j)], [("xsr", b4), ("xsrpad", b4)], "x")
                    for hb in range(2):
                        bk, pb, pk = bank()
                        TRG([(pb[:, q_ * 128:(q_ + 1) * 128], xsr[b4][:, (hb * 4 + q_) * 128:(hb * 4 + q_ + 1) * 128]) for q_ in range(4)],
                            r=[("xsr", b4), "cst"], w=[pk])
                        COPY("scalar" if hb == 0 else "vector", xTm3[:, hb * 4:(hb + 1) * 4, b4 * 128:(b4 + 1) * 128], v3(pb[:], 4), r=[pk], w=[("xTm", b4, hb)])
                    COPY("gpsimd", v3(wqs[j % 2], 4)[:, b4, :], xsr[b4][:, 1024:1028], r=[("xsr", b4), ("xsrpad", b4)], w=[("wqs", j % 2, b4)])
                XTMK = [("xTm", b4, hb) for b4 in range(4) for hb in range(2)]
                for q in range(4):
                    s_ = n_unit % 2
                    hs = n_unit % 2
                    hT3 = v3(hT[hs], 4)
                    wg3, wu3 = v3(WG(s_), 8), v3(WU(s_), 8)
                    for jc in range(4):
                        bg, pbg, pkg = bank()
                        bu, pbu, pku = bank()
                        MMG([(pbg[:, :], wg3[:, k, jc * 128:(jc + 1) * 128], xTm3[:, k, :], k == 0, k == 7, None) for k in range(8)],
                            r=[("W", s_ * 3 + 0, 0), ("W", s_ * 3 + 0, 1)] + XTMK, w=[pkg])
                        MMG([(pbu[:, :], wu3[:, k, jc * 128:(jc + 1) * 128], xTm3[:, k, :], k == 0, k == 7, None) for k in range(8)],
                            r=[("W", s_ * 3 + 1, 0), ("W", s_ * 3 + 1, 1)] + XTMK, w=[pku])
                        sgb = sg32[jc % 2]
                        ACT(sgb, pbg[:, :], AF.Silu, r=[pkg], w=[("sg", jc % 2)])
                        TT("vector", hT3[:, jc, :], sgb, pbu[:, :], ALU.mult, r=[("sg", jc % 2), pku], w=[("hT", hs, jc)])
                    if pend is not None:
                        down(*pend)
                    nxt = n_unit + 1
                    if nxt < nj * 4:
                        load_w(nxt // 4, nxt % 4, nxt % 2)
                    pend = (j, q, s_, hs)
                    n_unit += 1
            down(*pend)
            P.barrier()
            if l + 1 < n_layers:
                load_A_weights(l + 1)
                prefetched.add(l + 1)
            if use_ls:
                P.begin_capture()
            YSK = [("ys", j) for j in range(nj)]
            AF32.reset()
            NS4 = 7
            p4x = [AF32.alloc(1024) for _ in range(NS4)]
            p4y = [AF32.alloc(1024) for _ in range(NS4)]
            ln4 = [AF32.alloc(18) for _ in range(NS4)]
            def p4_issue(b):
                sl = b % NS4
                P.dma("sync", p4x[sl], src[0][b * 128:(b + 1) * 128, :], [(src[1], b)], [("p4x", sl)], "x")
                P.dma("gpsimd", None, None, YSK + ["slot_i"], [("p4y", sl)], "ix",
                      fn=(lambda e, sl=sl, b=b: e.indirect_dma_start(out=p4y[sl], out_offset=None, in_=ys_d[0:nj * 512, :],
                                                                  in_offset=bass.IndirectOffsetOnAxis(ap=slot_i[:, b:b + 1], axis=0))))
            for b in range(min(NS4 - 1, nbB)):
                p4_issue(b)
            for b in range(nbB):
                if b + NS4 - 1 < nbB:
                    p4_issue(b + NS4 - 1)
                sl = b % NS4
                kx = ("p4x", sl)
                xz = p4x[sl]
                for hf in range(2):
                    STT(xz[:, hf * 512:(hf + 1) * 512], xz[:, hf * 512:(hf + 1) * 512], ALPHA, p4y[sl][:, hf * 512:(hf + 1) * 512], ALU.mult, ALU.add,
                        r=[kx, ("p4y", sl)], w=[kx])
                layer_norm(xz, lnp[:, 0:D], lnp[:, D:2 * D], kx, eng_g="vector", eng_b="gpsimd", tmp=ln4[sl])
                P.dma("sync", dst[0][b * 128:(b + 1) * 128, :], xz, [kx], [(dst[1], b)], "st")
            if use_ls:
                P.end_capture()

        phases = dbg.get("phases") if dbg else None
        for l in range(n_layers):
            srcA = (x_in, "x_in") if l == 0 else (xb, "xb")
            lastA = phases is not None and phases == "A" and l == n_layers - 1
            dstA = (out, "out") if lastA else (xa, "xa")
            phase_A(l, srcA, dstA)
            P.barrier()
            if lastA:
                break
            dstB = (out, "out") if l == n_layers - 1 else (xb, "xb")
            (phase_B if (dbg and dbg.get("dense")) else phase_B2)(l, (xa, "xa"), dstB)
            P.barrier()
        P.emit(nc, st)
    return nc


def _col_perm():
    cols = []
    for j in range(4):
        cols += list(range(j * 64, j * 64 + 64)) + list(range((4 + j) * 64, (4 + j) * 64 + 64))
    cols += list(range(512, 640))
    cols += list(range(768, 1024))
    cols += list(range(1024, 1280))
    cols += list(range(1536, 1792))
    cols += list(range(1792, 2048))
    cols += list(range(640, 768))
    cols += list(range(1280, 1536))
    return np.array(cols)


def _row_perm():
    rows = []
    for j in range(4):
        rows += list(range(j * 64, j * 64 + 64)) + list(range((4 + j) * 64, (4 + j) * 64 + 64))
    rows += list(range(512, 1024))
    return np.array(rows)


def _t5_bucket(dist):
    d = np.maximum(dist, 0)
    large = 16 + (np.log(np.maximum(d, 1).astype(np.float32) / 16) / math.log(128 / 16) * 16).astype(np.int32)
    large = np.minimum(large, 31)
    return np.where(d < 16, d, large)


def _constants():
    c = np.zeros((128, 1024), np.float32)
    c[:, 0:128] = np.eye(128, dtype=np.float32)
    s_ = np.arange(128)[:, None]
    t_ = np.arange(128)[None, :]
    c[:, 128:256] = ((s_ <= t_) & (s_ // 16 == t_ // 16)).astype(np.float32)
    c[:, 256:384] = (t_ % 16 != 0).astype(np.float32)
    c[:, 384:512] = (s_ // 64 == t_ // 64).astype(np.float32) / 64.0
    c[:, 512:520] = (s_ // 16 == np.arange(8)[None, :]).astype(np.float32)
    c[:, 640:768] = (s_ < t_).astype(np.float32)
    c[:, 768:896] = 1.0
    c[:, 896] = 2.0 * np.arange(128)
    c[:, 897:905] = 512.0 * np.arange(8)[None, :]
    c[:, 905:905 + NJ] = np.arange(NJ)[None, :]
    return c


def _rows(w, nk):
    w = np.asarray(w, dtype=np.float32)
    n = w.shape[-1]
    w = w.reshape(L, NE, nk, 128, n).transpose(0, 1, 3, 2, 4)
    return np.ascontiguousarray(w).reshape(L * NE * 128 * 2, 2048)


def _prep(inputs):
    f = lambda a: np.ascontiguousarray(np.asarray(a, dtype=np.float32))
    cp, rp = _col_perm(), _row_perm()
    w_in = f(np.asarray(inputs["w_in"])[:, :, cp])
    b_in = np.asarray(inputs["b_in"], dtype=np.float32)[:, cp]
    w_out = f(np.asarray(inputs["w_out"])[:, rp, :])
    bfm = f(b_in[:, :NFM * 128].reshape(L, NFM, 128).transpose(2, 0, 1).reshape(128, L * NFM))
    btm = f(b_in[:, NFM * 128:])
    lbl = f(np.asarray(inputs["hgrn_lb_logits"], dtype=np.float32).reshape(L, 2, 128).transpose(2, 0, 1).reshape(128, L * 2))
    nw = np.asarray(inputs["hgrn_norm"], dtype=np.float32)
    normw = f(np.tile(nw, (1, 2)).T)
    sk = np.asarray(inputs["attn_sinks"], dtype=np.float32)
    sinks = f(np.repeat(sk.reshape(L, 2, 4), 64, axis=1).transpose(1, 0, 2).reshape(128, L * 4))
    lnp = f(np.stack([inputs["ln1_g"], inputs["ln1_b"], inputs["ln2_g"], inputs["ln2_b"]], axis=1))
    rb = f(np.asarray(inputs["router_bias"]).reshape(1, NE))
    rel = np.asarray(inputs["rel_bias"], dtype=np.float32)
    j = np.arange(128)[:, None]
    i = np.arange(128)[None, :]
    biasT = np.zeros((128, 2, 2, 4, 128), np.float32)
    maskf = np.zeros((128, 2, 2, 4, 128), np.float32)
    for pv in range(2):
        dist = (i + 128 - j) if pv == 0 else (i - j)
        bk = _t5_bucket(dist)
        inw = (dist >= 0) & (dist < 128)
        for g in range(2):
            for j4 in range(4):
                biasT[:, pv, g, j4, :] = rel[bk, g * 4 + j4]
                maskf[:, pv, g, j4, :] = inw
    shared = {
        "w_in": w_in, "w_out": w_out, "w_mem": f(inputs["w_mem_kv"]),
        "wgr": _rows(inputs["w_gate"], 8), "wur": _rows(inputs["w_up"], 8), "wdr": _rows(inputs["w_down"], 4),
        "w_router": f(inputs["w_router"]), "cst": _constants(),
        "biasT": f(biasT.reshape(128, 2048)), "maskf": f(maskf.reshape(128, 2048)),
        "bfm": bfm, "btm": btm, "lbl": lbl, "normw": normw, "sinks": sinks, "lnp": lnp, "rbias": rb,
    }
    return shared


_NC_CACHE = {}


def kernel(**inputs):
    shared = _prep(inputs)
    x = np.asarray(inputs["x"], dtype=np.float32)
    mem = np.asarray(inputs["mem"], dtype=np.float32)
    if "nc" not in _NC_CACHE:
        _NC_CACHE["nc"] = build_program()
    nc = _NC_CACHE["nc"]
    in_maps = []
    for c in range(NCORES):
        m = dict(shared)
        m["x"] = np.ascontiguousarray(x[c])
        m["mem"] = np.ascontiguousarray(mem[c])
        in_maps.append(m)
    res = run_bass_kernel_spmd(nc, in_maps, core_ids=list(range(NCORES)))
    return np.stack([r["out"] for r in res.results], axis=0)
```

```python
import math
import numpy as np
from contextlib import ExitStack
import concourse.bass as bass
import concourse.mybir as mybir
from concourse.bass_utils import run_bass_kernel_spmd

F32 = mybir.dt.float32
BF16 = mybir.dt.bfloat16
AF = mybir.ActivationFunctionType
ALU = mybir.AluOpType
AX = mybir.AxisListType

L = 4
D = 1024
S = 4096
NB = S // 128
MEM = 256
NE = 16
DE = 512
NCORES = 8
ALPHA = (2 * L) ** 0.25
LN_EPS = 1e-5
RMS_EPS = 1e-6
NFM = 13
TMW = 384
ENGINES = ["sync", "scalar", "vector", "gpsimd", "tensor"]

SAME_ENGINE_SYNC = True
SYNC_LAT = 250.0
TSUP = 1024
NJ = (S + 4 * 511) // 512
NSLOT = NJ * 512
XW = 1040
I32 = mybir.dt.int32
NBS = TSUP // 128


class Prog:
    def __init__(self):
        self.ops = {e: [] for e in ENGINES}
        self.cnt = {e: 0 for e in ENGINES}
        self.known = {e: {} for e in ENGINES}
        self.bufs = {}
        self.dma_pools = {}
        self.dma_rr = {}
        self.dma_cnt = {}
        self.enabled = True
        self.cap = None

    def add_pool(self, name, n):
        self.dma_pools[name] = [f"d_{name}{i}" for i in range(n)]
        self.dma_rr[name] = 0
        for k in self.dma_pools[name]:
            self.dma_cnt[k] = 0

    def _deps(self, eng, reads, writes):
        need = {}

        def add(tok):
            if tok is None:
                return
            k, v = tok
            if need.get(k, 0) < v:
                need[k] = v
        for b in reads:
            st = self.bufs.get(b)
            if st:
                add(st[0])
        for b in writes:
            st = self.bufs.get(b)
            if st:
                add(st[0])
                for t in st[1].values():
                    add(t)
        waits = []
        for k, v in need.items():
            if k == eng and (eng == "tensor" or not SAME_ENGINE_SYNC):
                continue
            if self.known[eng].get(k, 0) >= v:
                continue
            self.known[eng][k] = v
            waits.append((k, v))
        return waits

    def _commit(self, tok, reads, writes):
        for b in reads:
            st = self.bufs.setdefault(b, [None, {}])
            st[1][tok[0]] = tok
        for b in writes:
            self.bufs[b] = [tok, {}]

    def op(self, eng, fn, reads=(), writes=(), cost=200.0):
        if not self.enabled:
            return
        if self.cap is not None:
            self.cap.append(("op", eng, fn, list(reads), list(writes), cost, None))
            return
        ex = [b for b in reads if isinstance(b, tuple) and len(b) == 2 and b[0] == "ps"]
        if ex:
            reads = [b for b in reads if b not in ex]
            writes = list(writes) + ex
        waits = self._deps(eng, reads, writes)
        self.cnt[eng] += 1
        tok = (eng, self.cnt[eng])
        self.ops[eng].append((waits, fn, (eng, 1)))
        self._commit(tok, reads, writes)

    def dma(self, q, out, in_, reads, writes, pool, fn=None, cost=4000.0):
        if not self.enabled:
            return
        if self.cap is not None:
            self.cap.append(("dma", q, fn, list(reads), list(writes), cost, (out, in_, pool)))
            return
        sems = self.dma_pools[pool]
        i = self.dma_rr[pool]
        self.dma_rr[pool] = (i + 1) % len(sems)
        sk = sems[i]
        prev = self.dma_cnt[sk]
        waits = self._deps(q, reads, writes)
        if prev > 0 and self.known[q].get(sk, 0) < prev:
            self.known[q][sk] = prev
            waits.append((sk, prev))
        self.dma_cnt[sk] = prev + 16
        tok = (sk, prev + 16)
        if fn is None:
            fn = (lambda e, o=out, a=in_: e.dma_start(out=o, in_=a))
        self.ops[q].append((waits, fn, (sk, 16)))
        self._commit(tok, reads, writes)

    def begin_capture(self):
        self.cap = []

    def end_capture(self, sync_lat=None):
        sync_lat = SYNC_LAT if sync_lat is None else sync_lat
        ops, self.cap = self.cap, None
        n = len(ops)
        last_w, readers = {}, {}
        preds = [set() for _ in range(n)]
        for i, (kind, eng, fn, reads, writes, cost, extra) in enumerate(ops):
            ex = [b for b in reads if isinstance(b, tuple) and len(b) == 2 and b[0] == "ps"]
            rd = [b for b in reads if b not in ex]
            wr = list(writes) + ex
            for b in rd:
                if b in last_w:
                    preds[i].add(last_w[b])
            for b in wr:
                if b in last_w:
                    preds[i].add(last_w[b])
                for r_ in readers.get(b, ()):
                    preds[i].add(r_)
            for b in rd:
                readers.setdefault(b, []).append(i)
            for b in wr:
                last_w[b] = i
                readers[b] = []
            preds[i].discard(i)
        succs = [[] for _ in range(n)]
        npred = [len(p) for p in preds]
        for i, p in enumerate(preds):
            for j in p:
                succs[j].append(i)
        import heapq
        ready_t = [0.0] * n
        finish = [0.0] * n
        eng_free = {e: 0.0 for e in ENGINES}
        ready = {e: [] for e in ENGINES}
        for i in range(n):
            if npred[i] == 0:
                heapq.heappush(ready[ops[i][1]], i)
        order = []
        done = 0
        while done < n:
            best = None
            for e in ENGINES:
                cand = None
                for i in ready[e][:48]:
                    st = max(eng_free[e], ready_t[i])
                    if cand is None or (st, i) < cand[:2]:
                        cand = (st, i, e)
                if cand is not None and (best is None or cand[:2] < best[:2]):
                    best = cand
            st, i, e = best
            ready[e].remove(i)
            heapq.heapify(ready[e])
            kind, _, fn, reads, writes, cost, extra = ops[i]
            if kind == "dma":
                eng_free[e] = st + 150.0
            else:
                eng_free[e] = st + cost
            finish[i] = st + cost
            order.append(i)
            done += 1
            for j in succs[i]:
                ready_t[j] = max(ready_t[j], finish[i] + sync_lat)
                npred[j] -= 1
                if npred[j] == 0:
                    heapq.heappush(ready[ops[j][1]], j)
        for i in order:
            kind, eng, fn, reads, writes, cost, extra = ops[i]
            if kind == "op":
                self.op(eng, fn, reads, writes)
            else:
                self.dma(eng, extra[0], extra[1], reads, writes, extra[2], fn=fn)
        return max(finish) if n else 0.0

    def barrier(self):
        assert self.cap is None
        self.enabled = True
        cur = {e: self.cnt[e] for e in ENGINES}
        cur.update(self.dma_cnt)
        for e in ENGINES:
            waits = []
            for k, v in cur.items():
                if v > 0 and self.known[e].get(k, 0) < v:
                    if k == e and e == "tensor":
                        continue
                    self.known[e][k] = v
                    waits.append((k, v))
            self.ops[e].append((waits, None, None))
        self.bufs.clear()

    def emit(self, nc, st):
        keys = list(ENGINES) + list(self.dma_cnt.keys())
        sem = {k: st.enter_context(nc.semaphore(f"s_{k}")) for k in keys}
        block = st.enter_context(nc.Block())

        def mk(name):
            def body(e):
                for waits, fn, inc in self.ops[name]:
                    for k, v in waits:
                        e.wait_ge(sem[k], v)
                    if fn is not None:
                        ins = fn(e)
                        ins.then_inc(sem[inc[0]], inc[1])
            return body
        block.sync(mk("sync"))
        block.scalar(mk("scalar"))
        block.vector(mk("vector"))
        block.gpsimd(mk("gpsimd"))
        block.tensor(mk("tensor"))


class Arena:
    def __init__(self, tens, size):
        self.t = tens
        self.size = size
        self.off = 0

    def reset(self):
        self.off = 0

    def alloc(self, n):
        o = self.off
        self.off += n
        assert self.off <= self.size, (self.off, self.size)
        return self.t[:, o:o + n]


def v3(ap, a):
    return ap.rearrange("p (a b) -> p a b", a=a)


def bc(ap, shape):
    return ap.to_broadcast(list(shape))


def build_program(n_layers=L, dbg=None):
    nc = bass.Bass("TRN2", target_bir_lowering=False)
    P = Prog()
    P.add_pool("x", 8)
    P.add_pool("st", 6)
    P.add_pool("w", 6)
    P.add_pool("m", 4)
    P.add_pool("ix", 8)

    def din(name, shape):
        return nc.dram_tensor(name, list(shape), F32, kind="ExternalInput").ap()
    x_in = din("x", [S, D])
    mem_in = din("mem", [MEM, D])
    w_in = din("w_in", [L, D, 2048])
    w_out = din("w_out", [L, D, D])
    w_mem = din("w_mem", [L, D, 512])
    NWR = L * NE * 128 * 2
    wgr = din("wgr", [NWR, 2048])
    wur = din("wur", [NWR, 2048])
    wdr = din("wdr", [NWR, 2048])
    w_router = din("w_router", [D, NE])
    cst = din("cst", [128, 1024])
    biasT = din("biasT", [128, 2048])
    maskf = din("maskf", [128, 2048])
    bfm_in = din("bfm", [128, L * NFM])
    btm_in = din("btm", [L, TMW])
    lbl_in = din("lbl", [128, L * 2])
    normw_in = din("normw", [128, L])
    sink_in = din("sinks", [128, L * 4])
    ln_in = din("lnp", [L, 4, D])
    rb_in = din("rbias", [1, NE])
    out = nc.dram_tensor("out", [S, D], F32, kind="ExternalOutput").ap()
    xa = nc.dram_tensor("xa", [S, D], F32, kind="Internal").ap()
    xb = nc.dram_tensor("xb", [S, D], F32, kind="Internal").ap()
    xs_d = nc.dram_tensor("xs_d", [NSLOT, XW], F32, kind="Internal").ap()
    ys_d = nc.dram_tensor("ys_d", [NSLOT, D], F32, kind="Internal").ap()

    with ExitStack() as st:
        E = st.enter_context

        def sb(name, shape, dt=F32):
            return E(nc.sbuf_tensor(name, list(shape), dt))
        WBUF = sb("WBUF", [128, 28672], BF16)
        AF32 = Arena(sb("AF32", [128, 15360], F32), 15360)
        ABF = Arena(sb("ABF", [128, 20736], BF16), 20736)
        cst_sb = sb("cst_sb", [128, 1024])
        tri = cst_sb[:, 640:768]
        ones128 = cst_sb[:, 768:896]
        iota2p = cst_sb[:, 896:897]
        thr = cst_sb[:, 897:905]
        jiota = cst_sb[:, 905:905 + NJ]
        slot_i = sb("slot_i", [128, NB], I32)
        widx_i = sb("widx_i", [128, NJ * 8], I32)
        ident = cst_sb[:, 0:128]
        hgmask = cst_sb[:, 128:256]
        resetm = cst_sb[:, 256:384]
        onesbd = cst_sb[:, 384:512]
        cmask = cst_sb[:, 512:520]
        expB = sb("expB", [128, 2048])
        ones_bf = sb("ones_bf", [128, 64], BF16)
        bfm = sb("bfm_sb", [128, L * NFM])
        nbfm = sb("nbfm_sb", [128, L * NFM])
        btm_bc = sb("btm_bc", [128, TMW])
        lb = sb("lb_sb", [128, L * 2])
        oml = sb("oml_sb", [128, L * 2])
        noml = sb("noml_sb", [128, L * 2])
        lbE = sb("lbE_sb", [128, L * 2])
        lbtmp = sb("lbtmp", [128, 8])
        normw = sb("normw_sb", [128, L])
        sinkE = sb("sinkE_sb", [128, L * 4])
        lnp = sb("lnp_sb", [128, 2 * D])
        rb_bc = sb("rb_bc", [128, NE])
        wr_sb = sb("wr_sb", [128, 8 * NE])
        memT = sb("memT", [128, 8 * MEM], BF16)
        mkT = sb("mkT", [128, 2 * MEM], BF16)
        mv_sb = sb("mv_sb", [128, 2 * 256], BF16)
        Sst = sb("Sst", [128, 2 * 2 * 8 * 64])
        Sbf = sb("Sbf", [128, 2 * 2 * 8 * 128], BF16)
        rout = sb("rout", [128, NB * NE])
        runp = sb("runp", [128, 4])
        Cst = sb("Cst", [128, 2 * 3 * 64])
        Cbf = sb("Cbf", [128, 2 * 3 * 128], BF16)
        ps = [E(nc.psum_tensor(f"ps{b}", [128, 512], F32)) for b in range(8)]
        bank_rr = [0]

        def bank():
            b = bank_rr[0]
            bank_rr[0] = (b + 1) % 8
            return b, ps[b], ("ps", b)

        def fsz(ap):
            return float(ap.free_size())

        def ACT(out, in_, func, bias=0.0, scale=1.0, r=(), w=()):
            P.op("scalar", lambda e: e.activation(out=out, in_=in_, func=func, bias=bias, scale=scale), r, w, cost=220.0 + 0.85 * fsz(in_))

        def TT(eng, out, in0, in1, op, r=(), w=()):
            P.op(eng, lambda e: e.tensor_tensor(out=out, in0=in0, in1=in1, op=op), r, w, cost=(120.0 + 1.05 * fsz(out)) if eng == "vector" else (250.0 + 2.3 * fsz(out)))

        def TS(eng, out, in0, s1, s2, op0, op1=None, r=(), w=()):
            if op1 is None:
                P.op(eng, lambda e: e.tensor_scalar(out=out, in0=in0, scalar1=s1, scalar2=None, op0=op0), r, w, cost=(120.0 + 1.05 * fsz(out)) if eng == "vector" else (250.0 + 2.3 * fsz(out)))
            else:
                P.op(eng, lambda e: e.tensor_scalar(out=out, in0=in0, scalar1=s1, scalar2=s2, op0=op0, op1=op1), r, w, cost=(120.0 + 1.05 * fsz(out)) if eng == "vector" else (250.0 + 2.3 * fsz(out)))

        def STT(out, in0, scalar, in1, op0, op1, r=(), w=()):
            P.op("vector", lambda e: e.scalar_tensor_tensor(out=out, in0=in0, scalar=scalar, in1=in1, op0=op0, op1=op1), r, w, cost=120.0 + 1.05 * fsz(out))

        def RECIP(out, in_, r=(), w=()):
            P.op("vector", lambda e: e.reciprocal(out=out, in_=in_), r, w, cost=120.0 + 1.05 * fsz(out))

        def COPY(eng, out, in_, r=(), w=()):
            if eng == "scalar":
                P.op("scalar", lambda e: e.copy(out=out, in_=in_), r, w, cost=220.0 + 0.85 * fsz(out))
            else:
                P.op(eng, lambda e: e.tensor_copy(out=out, in_=in_), r, w, cost=(120.0 + 1.05 * fsz(out)) if eng == "vector" else (250.0 + 2.3 * fsz(out)))

        def MMG(mms, r=(), w=()):
            def fn(e):
                ins = None
                for (o, l_, r_, s0, s1, tp) in mms:
                    if tp is None:
                        ins = e.matmul(o, l_, r_, start=s0, stop=s1)
                    else:
                        ins = e.matmul(o, l_, r_, start=s0, stop=s1, tile_position=tp)
                return ins
            P.op("tensor", fn, r, w, cost=60.0 + sum(max(64.0, fsz(m_[2])) * 0.45 for m_ in mms))

        def TRG(trs, r=(), w=()):
            def fn(e):
                ins = None
                for (o, i_) in trs:
                    ins = e.transpose(o, i_, ident)
                return ins
            P.op("tensor", fn, r, w, cost=60.0 + 110.0 * len(trs))

        def MEMSET(eng, ap, val, w=()):
            P.op(eng, lambda e: e.memset(ap, val), (), w)

        P.dma("sync", cst_sb[:], cst, (), ["cst"], "m")
        P.dma("sync", expB[:], biasT, (), ["expB"], "m")
        mtmp = AF32.alloc(2048)
        P.dma("sync", mtmp, maskf, (), ["mtmp"], "m")
        P.dma("sync", bfm[:], bfm_in, (), ["bfm"], "m")
        P.dma("sync", lbE[:], lbl_in, (), ["lbE"], "m")
        P.dma("sync", normw[:], normw_in, (), ["normw"], "m")
        P.dma("sync", sinkE[:], sink_in, (), ["sinkE"], "m")
        P.dma("sync", rb_bc[:], rb_in.partition_broadcast(128), (), ["rb"], "m")
        P.dma("sync", v3(wr_sb[:], 8), w_router.rearrange("(k p) n -> p k n", p=128), (), ["wr"], "m")
        ztile = AF32.alloc(4 * XW)
        MEMSET("gpsimd", ztile, 0.0, ["ztile"])
        for j_ in range(NJ):
            P.dma("sync", xs_d[j_ * 512:(j_ + 1) * 512, :].rearrange("(a p) w -> p a w", p=128), v3(ztile, 4), ["ztile"], [("xsz", j_)], "st")
        MEMSET("vector", ones_bf[:], 1.0, ["ones"])
        MEMSET("vector", Sst[:], 0.0, ["Sst"])
        MEMSET("gpsimd", Sbf[:], 0.0, ["Sbf"])
        MEMSET("vector", Cst[:], 0.0, ["Cst"])
        MEMSET("gpsimd", Cbf[:], 0.0, ["Cbf"])
        ACT(expB[:], expB[:], AF.Exp, r=["expB"], w=["expB"])
        TT("vector", expB[:], expB[:], mtmp, ALU.mult, r=["expB", "mtmp"], w=["expB"])
        ACT(sinkE[:], sinkE[:], AF.Exp, r=["sinkE"], w=["sinkE"])
        TS("vector", nbfm[:], bfm[:], -1.0, None, ALU.mult, r=["bfm"], w=["nbfm"])
        ACT(lbE[:], lbE[:], AF.Exp, r=["lbE"], w=["lbE"])
        lbE3 = v3(lbE[:], L)
        tot = lbtmp[:, 0:2]
        rtot = lbtmp[:, 2:4]
        cum = lbtmp[:, 4:6]
        TT("vector", tot, lbE3[:, 0, :], lbE3[:, 1, :], ALU.add, r=["lbE"], w=["lbtmp"])
        for l_ in range(2, L):
            TT("vector", tot, tot, lbE3[:, l_, :], ALU.add, r=["lbE", "lbtmp"], w=["lbtmp"])
        RECIP(rtot, tot, r=["lbtmp"], w=["lbtmp"])
        lb3 = v3(lb[:], L)
        MEMSET("vector", lb3[:, 0, :], 0.0, ["lb"])
        for l_ in range(1, L):
            if l_ == 1:
                COPY("vector", cum, lbE3[:, 1, :], r=["lbE", "lbtmp"], w=["lbtmp"])
            else:
                TT("vector", cum, cum, lbE3[:, l_, :], ALU.add, r=["lbE", "lbtmp"], w=["lbtmp"])
            TT("vector", lb3[:, l_, :], cum, rtot, ALU.mult, r=["lbtmp", "lb"], w=["lb"])
        TS("vector", oml[:], lb[:], -1.0, 1.0, ALU.mult, ALU.add, r=["lb"], w=["oml"])
        TS("vector", noml[:], oml[:], -1.0, None, ALU.mult, r=["oml"], w=["noml"])
        for mc in range(2):
            mt = AF32.alloc(1024)
            P.dma("sync", mt, mem_in[mc * 128:(mc + 1) * 128, :], (), [("mt", mc)], "x")
            for hb in range(2):
                b, pb, pk = bank()
                TRG([(pb[:, q * 128:(q + 1) * 128], mt[:, (hb * 4 + q) * 128:(hb * 4 + q + 1) * 128]) for q in range(4)],
                    r=[("mt", mc), "cst"], w=[pk])
                dst = v3(memT[:], 8)[:, hb * 4:(hb + 1) * 4, mc * 128:(mc + 1) * 128]
                COPY("vector", dst, v3(pb[:], 4), r=[pk], w=["memT"])
        P.barrier()

        def layer_norm(zt, gsl, bsl, key, eng_g="gpsimd", eng_b="gpsimd", tmp=None):
            if tmp is None:
                tmp = AF32.alloc(18)
            stats, mvv, sm = tmp[:, 0:12], tmp[:, 12:14], tmp[:, 14:18]
            for h in range(2):
                P.op("vector", lambda e, h=h: e.bn_stats(out=stats[:, h * 6:(h + 1) * 6], in_=zt[:, h * 512:(h + 1) * 512]),
                     [key], [(key, "_st")])
            P.op("vector", lambda e: e.bn_aggr(out=mvv, in_=stats), [(key, "_st")], [(key, "_mv")])
            ACT(sm[:, 0:1], mvv[:, 1:2], AF.Ln, bias=LN_EPS, r=[(key, "_mv")], w=[(key, "_sm")])
            ACT(sm[:, 1:2], sm[:, 0:1], AF.Exp, scale=-0.5, r=[(key, "_sm")], w=[(key, "_sm")])
            TS("vector", sm[:, 2:3], mvv[:, 0:1], -1.0, sm[:, 1:2], ALU.mult, ALU.mult, r=[(key, "_mv"), (key, "_sm")], w=[(key, "_sm2")])
            ACT(zt, zt, AF.Identity, bias=sm[:, 2:3], scale=sm[:, 1:2], r=[key, (key, "_sm"), (key, "_sm2")], w=[key])
            TT(eng_g, zt, zt, gsl, ALU.mult, r=[key, "lnp"], w=[key])
            TT(eng_b, zt, zt, bsl, ALU.add, r=[key, "lnp"], w=[key])

        def load_ln(l, which):
            for j in range(2):
                P.dma("sync", lnp[:, j * D:(j + 1) * D], ln_in[l, 2 * which + j:2 * which + j + 1, :].partition_broadcast(128), (), ["lnp"], "m")


        def route_block(b, xtile, kxl, xT32b, ktl, R, rs, kr, ing3, posb3, base3, wq3, run, wr3):
            xT32_3 = v3(xT32b, 8)
            for hb in range(2):
                bk, pb, pk = bank()
                TRG([(pb[:, q * 128:(q + 1) * 128], xtile[:, (hb * 4 + q) * 128:(hb * 4 + q + 1) * 128]) for q in range(4)],
                    r=kxl + ["cst"], w=[pk])
                COPY("scalar" if hb == 0 else "vector", xT32_3[:, hb * 4:(hb + 1) * 4, :], v3(pb[:], 4), r=[pk], w=[ktl[hb]])
            bk, pb, pk = bank()
            MMG([(pb[:, 0:NE], xT32_3[:, k, :], wr3[:, k, :], k == 0, k == 7, None) for k in range(8)], r=ktl + ["wr"], w=[pk])
            ACT(R["en"], pb[:, 0:NE], AF.Exp, scale=-1.0, r=[pk], w=[kr])
            TS("vector", R["en"], R["en"], 1.0, None, ALU.add, r=[kr], w=[kr])
            RECIP(R["sc"], R["en"], r=[kr], w=[kr])
            TT("vector", R["sel"], R["sc"], rb_bc[:], ALU.add, r=[kr, "rb"], w=[kr])
            sel3 = v3(R["sel"], 4)
            m1, m2, gsum, gmax, wsum, rws = rs[:, 0:4], rs[:, 4:8], rs[:, 8:12], rs[:, 12:13], rs[:, 20:21], rs[:, 21:22]
            ing = ing3[:, b, :]
            P.op("vector", lambda e: e.tensor_reduce(out=m1, in_=sel3, axis=AX.X, op=ALU.max), [kr], [kr])
            TT("vector", v3(R["eq"], 4), sel3, bc(m1.unsqueeze(2), [128, 4, 4]), ALU.is_equal, r=[kr], w=[kr])
            STT(R["sel2"], R["eq"], -1.0e9, R["sel"], ALU.mult, ALU.add, r=[kr], w=[kr])
            P.op("vector", lambda e: e.tensor_reduce(out=m2, in_=v3(R["sel2"], 4), axis=AX.X, op=ALU.max), [kr], [kr])
            TT("vector", gsum, m1, m2, ALU.add, r=[kr], w=[kr])
            P.op("vector", lambda e: e.tensor_reduce(out=gmax, in_=gsum, axis=AX.X, op=ALU.max), [kr], [kr])
            TS("vector", ing, gsum, gmax, None, ALU.is_equal, r=[kr], w=[kr, ("ing", b)])
            TT("vector", v3(R["ge"], 4), sel3, bc(m2.unsqueeze(2), [128, 4, 4]), ALU.is_ge, r=[kr], w=[kr])
            TT("vector", v3(R["selm"], 4), v3(R["ge"], 4), bc(ing.unsqueeze(2), [128, 4, 4]), ALU.mult, r=[kr, ("ing", b)], w=[kr])
            TT("vector", R["wsel"], R["sc"], R["selm"], ALU.mult, r=[kr], w=[kr])
            P.op("vector", lambda e: e.tensor_reduce(out=wsum, in_=R["wsel"], axis=AX.X, op=ALU.add), [kr], [kr])
            RECIP(rws, wsum, r=[kr], w=[kr])
            TS("vector", R["wtb"], R["wsel"], rws, None, ALU.mult, r=[kr], w=[kr])
            P.op("vector", lambda e: e.tensor_reduce(out=wq3[:, b, :], in_=R["wtb"].rearrange("p (g q) -> p q g", g=4), axis=AX.X, op=ALU.add),
                 [kr], [("wq", b)])
            bk2, pb2, pk2 = bank()
            MMG([(pb2[:, 0:4], tri, ing, True, True, None), (pb2[:, 4:8], ones128, ing, True, True, None)], r=[("ing", b), "cst"], w=[pk2])
            COPY("vector", posb3[:, b, :], pb2[:, 0:4], r=[pk2], w=[("posb", b)])
            COPY("vector", base3[:, b, :], run, r=["run"], w=[("base", b)])
            TT("vector", run, run, pb2[:, 4:8], ALU.add, r=["run", pk2], w=["run"])

        prefetched = set()

        def load_A_weights(l):
            winv = v3(WBUF[:, 0:16384], 8)
            woutv = v3(WBUF[:, 16384:24576], 8)
            wmemv = v3(WBUF[:, 24576:28672], 8)
            for k2 in range(4):
                P.dma("gpsimd", winv[:, 2 * k2:2 * k2 + 2, :], w_in[l].rearrange("(k p) n -> p k n", p=128)[:, 2 * k2:2 * k2 + 2, :], (), [("W", k2)], "w")
            P.dma("gpsimd", wmemv, w_mem[l].rearrange("(k p) n -> p k n", p=128), (), [("W", 6)], "w")
            for k2 in range(2):
                P.dma("gpsimd", woutv[:, 4 * k2:4 * k2 + 4, :], w_out[l].rearrange("(k p) n -> p k n", p=128)[:, 4 * k2:4 * k2 + 4, :], (), [("W", 4 + k2)], "w")

        def phase_A(l, src, dst):
            AF32.reset()
            ABF.reset()
            winv = v3(WBUF[:, 0:16384], 8)
            woutv = v3(WBUF[:, 16384:24576], 8)
            wmemv = v3(WBUF[:, 24576:28672], 8)
            if l not in prefetched:
                load_A_weights(l)
            P.dma("sync", btm_bc[:], btm_in[l:l + 1, :].partition_broadcast(128), (), ["btm"], "m")
            load_ln(l, 0)
            WIN = [("W", i) for i in range(4)]
            WOUT = [("W", 4), ("W", 5)]
            memT3 = v3(memT[:], 8)
            mkT3 = v3(mkT[:], 2)
            for pr in range(2):
                b, pb, pk = bank()
                MMG([(pb[:, 0:256], wmemv[:, k, pr * 128:(pr + 1) * 128], memT3[:, k, :], k == 0, k == 7, None) for k in range(8)],
                    r=[("W", 6), "memT"], w=[pk])
                COPY("vector", mkT3[:, pr, :], pb[:, 0:256], r=[pk], w=["mkT"])
            mv3 = v3(mv_sb[:], 2)
            for mc in range(2):
                b, pb, pk = bank()
                MMG([(pb[:, 0:256], memT3[:, k, mc * 128:(mc + 1) * 128], wmemv[:, k, 256:512], k == 0, k == 7, None) for k in range(8)],
                    r=[("W", 6), "memT"], w=[pk])
                COPY("scalar", mv3[:, mc, :], pb[:, 0:256], r=[pk], w=["mv"])

            xt = [AF32.alloc(1024) for _ in range(2)]
            pfs = [AF32.alloc(NFM * 128) for _ in range(2)]
            denss = [AF32.alloc(512) for _ in range(2)]
            lnts = [AF32.alloc(18) for _ in range(2)]
            xT32r = AF32.alloc(1024)
            wr3 = v3(wr_sb[:], 8)
            mrecs = [AF32.alloc(256) for _ in range(2)]
            hg = {}
            for nm in ["e1", "logf", "kk", "bcum", "eb", "enb", "eq", "qs", "kt32", "kh32", "osb", "lnms", "eg"]:
                hg[nm] = [AF32.alloc(256) for _ in range(2)]
            xT = [ABF.alloc(1024) for _ in range(2)]
            qTs = [ABF.alloc(512) for _ in range(2)]
            kT = [ABF.alloc(128) for _ in range(3)]
            swaV = [ABF.alloc(128) for _ in range(3)]
            hgVs = [ABF.alloc(256) for _ in range(2)]
            mqTs = [ABF.alloc(256) for _ in range(2)]
            Pts = [ABF.alloc(2048) for _ in range(2)]
            mixT = [ABF.alloc(1024) for _ in range(2)]
            PmTs = [ABF.alloc(1024) for _ in range(2)]
            qtTs = [ABF.alloc(256) for _ in range(2)]
            ktTs = [ABF.alloc(256) for _ in range(2)]
            khTs = [ABF.alloc(256) for _ in range(2)]
            qbds = [ABF.alloc(512) for _ in range(2)]
            for sl_ in range(2):
                MEMSET("gpsimd", qbds[sl_], 0.0, [(("hg", sl_), "qbd0"), (("hg", sl_), "qbd1")])
            Vblks = [ABF.alloc(2048) for _ in range(2)]
            scTs = [ABF.alloc(512) for _ in range(2)]
            Sst5 = Sst[:].rearrange("p (a b c d) -> p a b c d", a=2, b=2, c=8)
            Sbf5 = Sbf[:].rearrange("p (a b c d) -> p a b c d", a=2, b=2, c=8)
            Cst4 = Cst[:].rearrange("p (a b d) -> p a b d", a=2, b=3)
            Cbf4 = Cbf[:].rearrange("p (a b d) -> p a b d", a=2, b=3)
            bfm3 = v3(bfm[:], L)
            lb3_ = v3(lb[:], L)
            oml3 = v3(oml[:], L)
            noml3 = v3(noml[:], L)
            sinkE3 = v3(sinkE[:], L)
            expB4 = expB[:].rearrange("p (a b c) -> p a b c", a=2, b=2)

            def hgrn_block(i, sl, pf3, hgV, Vblk, scT, mixT3, km):
                par = i % 2
                H = {k_: v_[sl] for k_, v_ in hg.items()}
                qtT, ktT, khT, qbd = qtTs[sl], ktTs[sl], khTs[sl], qbds[sl]
                kp = ("hg", sl)
                kv = ("hgV", sl)
                Q2, F2, G2 = pf3[:, 5:7, :], pf3[:, 7:9, :], pf3[:, 9:11, :]
                rq, rf, rg = [("pf", sl, 4)], [("pf", sl, 4), ("pf", sl, 8)], [("pf", sl, 8)]
                A2 = lambda nm: v3(H[nm], 2)

                def sigm(dst, src3, rr, key):
                    ACT(A2(dst), src3, AF.Exp, scale=-1.0, r=rr, w=[key])
                    ACT(H[dst], H[dst], AF.Ln, bias=1.0, r=[key], w=[key])
                    ACT(H[dst], H[dst], AF.Exp, scale=-1.0, r=[key], w=[key])
                sigm("e1", F2, rf, (kp, "e1"))
                sigm("eq", Q2, rq, (kp, "eq"))
                sigm("eg", G2, rg, (kp, "eg"))
                for pr in range(2):
                    sl_ = slice(pr * 128, (pr + 1) * 128)
                    ACT(H["logf"][:, sl_], H["e1"][:, sl_], AF.Ln, bias=lb3_[:, l, pr:pr + 1], scale=oml3[:, l, pr:pr + 1], r=[(kp, "e1"), "lb", "oml"], w=[(kp, "logf", pr)])
                    TS("gpsimd", H["kk"][:, sl_], H["e1"][:, sl_], noml3[:, l, pr:pr + 1], oml3[:, l, pr:pr + 1], ALU.mult, ALU.add, r=[(kp, "e1"), "oml", "noml"], w=[(kp, "kk", pr)])
                    P.op("vector", lambda e, H=H, sl_=sl_: e.tensor_tensor_scan(out=H["bcum"][:, sl_], data0=resetm, data1=H["logf"][:, sl_], initial=0.0, op0=ALU.mult, op1=ALU.add),
                         [(kp, "logf", pr), "cst"], [(kp, "bcum", pr)], cost=400.0)
                BK = [(kp, "bcum", 0), (kp, "bcum", 1)]
                ACT(H["eb"], H["bcum"], AF.Exp, r=BK, w=[(kp, "eb")])
                ACT(H["enb"], H["bcum"], AF.Exp, scale=-1.0, r=BK, w=[(kp, "enb")])
                TT("gpsimd", A2("qs"), Q2, A2("eq"), ALU.mult, r=rq + [(kp, "eq")], w=[(kp, "qs")])
                TT("gpsimd", H["kt32"], H["kk"], H["enb"], ALU.mult, r=[(kp, "kk", 0), (kp, "kk", 1), (kp, "enb")], w=[(kp, "kt32")])
                STT(qtT, H["qs"], 0.125, H["eb"], ALU.mult, ALU.mult, r=[(kp, "qs"), (kp, "eb")], w=[(kp, "qtT")])
                COPY("gpsimd", ktT, H["kt32"], r=[(kp, "kt32")], w=[(kp, "ktT")])
                eb4 = H["eb"].rearrange("p (a c t) -> p a c t", a=2, c=8)
                TT("vector", v3(H["kh32"], 16), v3(H["kt32"], 16), bc(v3(H["eb"], 16)[:, :, 15:16], [128, 16, 16]), ALU.mult,
                   r=[(kp, "kt32"), (kp, "eb")], w=[(kp, "kh32")])
                qtT3 = v3(qtT, 2)
                qbd4 = qbd.rearrange("p (a r t) -> p a r t", a=2, r=2)
                COPY("gpsimd", qbd4[0:64, :, 0, :], qtT3[0:64, :, :], r=[(kp, "qtT")], w=[(kp, "qbd0")])
                COPY("gpsimd", qbd4[64:128, :, 1, :], qtT3[64:128, :, :], r=[(kp, "qtT")], w=[(kp, "qbd1")])
                bt, pbt, pkt = bank()
                TRG([(pbt[:, pr * 128:(pr + 1) * 128], H["kh32"][:, pr * 128:(pr + 1) * 128]) for pr in range(2)], r=[(kp, "kh32"), "cst"], w=[pkt])
                COPY("scalar", khT, pbt[:, 0:256], r=[pkt], w=[(kp, "khT")])
                Vb4 = Vblk.rearrange("p (h c v) -> p h c v", h=4, c=8)
                hgV3 = v3(hgV, 4)
                khT3 = v3(khT, 2)
                for h in range(4):
                    TT("gpsimd", Vb4[:, h, :, :], bc(hgV3[:, h, :].unsqueeze(1), [128, 8, 64]), bc(cmask.unsqueeze(2), [128, 8, 64]), ALU.mult,
                       r=[kv, "cst"], w=[("Vblk", sl, h)])
                pbus = []
                for pr in range(2):
                    bu, pbu, pku = bank()
                    pbus.append((pbu, pku))
                    mmu = []
                    for r_ in range(2):
                        h = 2 * pr + r_
                        mmu.append((pbu[r_ * 64:(r_ + 1) * 64, :], khT3[:, pr, r_ * 64:(r_ + 1) * 64],
                                    Vb4[:, h, :, :].rearrange("p c v -> p (c v)"), True, True, (0, r_ * 64)))
                    MMG(mmu, r=[(kp, "khT"), ("Vblk", sl, 2 * pr), ("Vblk", sl, 2 * pr + 1)], w=[pku])
                bsc, pbsc, pksc = bank()
                scT3 = v3(scT, 4)
                ktT3 = v3(ktT, 2)
                MMG([(pbsc[:, pr * 256:(pr + 1) * 256], ktT3[:, pr, :], qbd4[:, pr, :, :].rearrange("p r t -> p (r t)"), True, True, None) for pr in range(2)],
                    r=[(kp, "ktT"), (kp, "qbd0"), (kp, "qbd1")], w=[pksc])
                TT("vector", scT3, v3(pbsc[:, :], 4), bc(hgmask.unsqueeze(1), [128, 4, 128]), ALU.mult, r=[pksc, "cst"], w=[("scT", sl)])
                cin, cout = i % 3, (i + 1) % 3
                for pr in range(2):
                    pbu, pku = pbus[pr]
                    pu3 = v3(pbu[:], 8)
                    for c in range(8):
                        s_prev = Cst4[:, pr, cin, :] if c == 0 else Sst5[:, pr, par, c, :]
                        s_out = Cst4[:, pr, cout, :] if c == 7 else Sst5[:, pr, par, c + 1, :]
                        STT(s_out, s_prev, eb4[:, pr, c, 15:16], pu3[:, c, :], ALU.mult, ALU.add,
                            r=[pku, (kp, "eb"), ("Sst", pr, par), ("Cst", pr, cin)], w=[("Sst", pr, par)] + ([("Cst", pr, cout)] if c == 7 else []))
                    COPY("scalar", Sbf5[0:64, pr, par, 1:8, 0:64], Sst5[0:64, pr, par, 1:8, :], r=[("Sst", pr, par)], w=[("Sbf", pr, par, 0)])
                    COPY("scalar", Sbf5[64:128, pr, par, 1:8, 64:128], Sst5[64:128, pr, par, 1:8, :], r=[("Sst", pr, par)], w=[("Sbf", pr, par, 1)])
                    COPY("gpsimd", Cbf4[0:64, pr, cout, 0:64], Cst4[0:64, pr, cout, :], r=[("Cst", pr, cout)], w=[("Cbf", pr, cout, 0)])
                    COPY("gpsimd", Cbf4[64:128, pr, cout, 64:128], Cst4[64:128, pr, cout, :], r=[("Cst", pr, cout)], w=[("Cbf", pr, cout, 1)])
                bo, pbo, pko = bank()
                mmo = []
                for pr in range(2):
                    for r_ in range(2):
                        h = 2 * pr + r_
                        mmo.append((pbo[r_ * 64:(r_ + 1) * 64, pr * 128:(pr + 1) * 128], hgV3[:, h, :], scT3[:, h, :], True, False, (0, r_ * 64)))
                    for c in range(8):
                        sb_prev = Cbf4[:, pr, cin, :] if c == 0 else Sbf5[:, pr, par, c, :]
                        mmo.append((pbo[:, pr * 128 + c * 16:pr * 128 + (c + 1) * 16], sb_prev, qtT3[:, pr, c * 16:(c + 1) * 16], False, c == 7, None))
                rs_ = [kv, ("scT", sl), (kp, "qtT")]
                for pr in range(2):
                    rs_ += [("Sbf", pr, par, 0), ("Sbf", pr, par, 1), ("Cbf", pr, cin, 0), ("Cbf", pr, cin, 1)]
                MMG(mmo, r=rs_, w=[pko])
                COPY("scalar", H["osb"], pbo[:, 0:256], r=[pko], w=[(kp, "osb")])
                ACT(H["kh32"], pbo[:, 0:256], AF.Square, r=[pko], w=[(kp, "kh32")])
                bm, pbm, pkm = bank()
                MMG([(pbm[:, 0:256], onesbd, H["kh32"], True, True, None)], r=[(kp, "kh32"), "cst"], w=[pkm])
                TT("gpsimd", A2("eg"), G2, A2("eg"), ALU.mult, r=rg + [(kp, "eg")], w=[(kp, "eg")])
                ACT(H["lnms"], pbm[:, 0:256], AF.Ln, bias=RMS_EPS, r=[pkm], w=[(kp, "lnms")])
                ACT(H["lnms"], H["lnms"], AF.Exp, scale=-0.5, r=[(kp, "lnms")], w=[(kp, "lnms")])
                STT(H["osb"], H["osb"], normw[:, l:l + 1], H["lnms"], ALU.mult, ALU.mult, r=[(kp, "osb"), (kp, "lnms"), "normw"], w=[(kp, "osb")])
                TT("gpsimd", mixT3[:, 4:6, :], A2("osb"), A2("eg"), ALU.mult, r=[(kp, "osb"), (kp, "eg")], w=[(km, 1, 0), (km, 1, 1)])

            def blockgen(i):
                sl = i % 2
                s3 = i % 3
                p3 = (i - 1) % 3
                kx = ("xt", sl)
                pf3 = v3(pfs[sl], NFM)
                qT, hgV, mqT, Pt, PmT = qTs[sl], hgVs[sl], mqTs[sl], Pts[sl], PmTs[sl]
                dens, mrec = denss[sl], mrecs[sl]
                rec = dens
                P.dma("sync", xt[sl], src[0][i * 128:(i + 1) * 128, :], [(src[1], i)], [kx], "x")
                xT3 = v3(xT[sl], 8)
                for hb in range(2):
                    b, pb, pk = bank()
                    TRG([(pb[:, q * 128:(q + 1) * 128], xt[sl][:, (hb * 4 + q) * 128:(hb * 4 + q + 1) * 128]) for q in range(4)],
                        r=[kx, "cst"], w=[pk])
                    COPY("scalar", xT3[:, hb * 4:(hb + 1) * 4, :], v3(pb[:], 4), r=[pk], w=[("xT", sl, hb)])
                yield
                XTK = [("xT", sl, 0), ("xT", sl, 1)]
                for c0 in range(0, NFM, 4):
                    nch = min(4, NFM - c0)
                    b, pb, pk = bank()
                    mms = []
                    for c in range(c0, c0 + nch):
                        for k in range(8):
                            mms.append((pb[:, (c - c0) * 128:(c - c0 + 1) * 128], winv[:, k, c * 128:(c + 1) * 128], xT3[:, k, :], k == 0, k == 7, None))
                    MMG(mms, r=WIN + XTK, w=[pk])
                    TT("vector", pf3[:, c0:c0 + nch, :], v3(pb[:, 0:nch * 128], nch),
                       bc(bfm3[:, l, c0:c0 + nch].unsqueeze(2), [128, nch, 128]), ALU.add, r=[pk, "bfm"], w=[("pf", sl, c0)])
                    yield
                b, pb, pk = bank()
                MMG([(pb[:, 0:TMW], xT3[:, k, :], winv[:, k, NFM * 128:2048], k == 0, k == 7, None) for k in range(8)], r=WIN + XTK, w=[pk])
                TT("vector", swaV[s3], pb[:, 0:128], btm_bc[:, 0:128], ALU.add, r=[pk, "btm"], w=[("swaV", s3)])
                TT("vector", hgV, pb[:, 128:384], btm_bc[:, 128:384], ALU.add, r=[pk, "btm"], w=[("hgV", sl)])
                COPY("scalar", v3(qT, 4), pf3[:, 0:4, :], r=[("pf", sl, 0)], w=[("qT", sl)])
                COPY("gpsimd", kT[s3], pf3[:, 4, :], r=[("pf", sl, 4)], w=[("kT", s3)])
                COPY("gpsimd", v3(mqT, 2), pf3[:, 11:13, :], r=[("pf", sl, 8), ("pf", sl, 12)], w=[("mqT", sl)])
                yield
                qT3 = v3(qT, 4)
                Pt4 = Pt.rearrange("p (a b c) -> p a b c", a=2, b=2)
                pvs = [1] if i == 0 else [0, 1]
                for g in range(2):
                    for pv in pvs:
                        ksl = s3 if pv == 1 else p3
                        b, pb, pk = bank()
                        MMG([(pb[:, :], kT[ksl][g * 64:(g + 1) * 64, :], qT3[g * 64:(g + 1) * 64, :, :], True, True, None)],
                            r=[("kT", ksl), ("qT", sl)], w=[pk])
                        ACT(Pt4[:, pv, g, :], pb[:, :], AF.Exp, scale=0.125, r=[pk], w=[("Pt", sl, pv, g)])
                        TT("vector" if g == 0 else "gpsimd", Pt4[:, pv, g, :], Pt4[:, pv, g, :], expB4[:, pv, g, :], ALU.mult,
                           r=[("Pt", sl, pv, g), "expB"], w=[("Pt", sl, pv, g)])
                        yield
                bo, pbo, pko = bank()
                bd, pbd, pkd = bank()
                mmo, mmd = [], []
                for g in range(2):
                    for n_, pv in enumerate(pvs):
                        ksl = s3 if pv == 1 else p3
                        mmo.append((pbo[g * 64:(g + 1) * 64, :], swaV[ksl][:, g * 64:(g + 1) * 64], Pt4[:, pv, g, :], n_ == 0, n_ == len(pvs) - 1, (0, g * 64)))
                        mmd.append((pbd[g * 64:(g + 1) * 64, :], ones_bf[:, :], Pt4[:, pv, g, :], n_ == 0, n_ == len(pvs) - 1, (0, g * 64)))
                rr = [("Pt", sl, pv, g) for pv in pvs for g in range(2)] + [("swaV", s3), ("swaV", p3), "ones"]
                MMG(mmo, r=rr, w=[pko])
                MMG(mmd, r=rr, w=[pkd])
                yield
                TT("vector", v3(dens, 4), v3(pbd[:], 4), bc(sinkE3[:, l, :].unsqueeze(2), [128, 4, 128]), ALU.add, r=[pkd, "sinkE"], w=[("dens", sl)])
                RECIP(rec, dens, r=[("dens", sl)], w=[("dens", sl)])
                mixT3 = v3(mixT[sl], 8)
                km = ("mixT", sl)
                TT("vector", mixT3[:, 0:4, :], v3(pbo[:], 4), v3(rec, 4), ALU.mult, r=[pko, ("dens", sl)], w=[(km, 0)])
                yield
                mqT3 = v3(mqT, 2)
                PmT4 = PmT.rearrange("p (r a b c) -> p r a b c", r=2, a=2, b=2)
                for r_ in range(2):
                    b, pb, pk = bank()
                    mms = []
                    for mc in range(2):
                        for pr in range(2):
                            mms.append((pb[:, (mc * 2 + pr) * 128:(mc * 2 + pr + 1) * 128], mkT3[r_ * 64:(r_ + 1) * 64, pr, mc * 128:(mc + 1) * 128],
                                        mqT3[r_ * 64:(r_ + 1) * 64, pr, :], True, True, None))
                    MMG(mms, r=["mkT", ("mqT", sl)], w=[pk])
                    ACT(PmT4[:, r_].rearrange("p a b c -> p (a b c)"), pb[:, :], AF.Exp, scale=0.125, r=[pk], w=[("PmT", sl, r_)])
                    yield
                bo, pbo, pko = bank()
                mmo = []
                for kind in range(2):
                    for h in range(4):
                        pr, r_ = h // 2, h % 2
                        for mc in range(2):
                            lhs = mv3[:, mc, h * 64:(h + 1) * 64] if kind == 0 else ones_bf[:, :]
                            mmo.append((pbo[r_ * 64:(r_ + 1) * 64, kind * 256 + pr * 128:kind * 256 + (pr + 1) * 128], lhs,
                                        PmT4[:, r_, mc, pr, :], mc == 0, mc == 1, (0, r_ * 64)))
                MMG(mmo, r=[("PmT", sl, 0), ("PmT", sl, 1), "mv", "ones"], w=[pko])
                RECIP(mrec, pbo[:, 256:512], r=[pko], w=[("mrec", sl)])
                TT("vector", mixT3[:, 6:8, :], v3(pbo[:, 0:256], 2), v3(mrec, 2), ALU.mult, r=[pko, ("mrec", sl)], w=[(km, 2)])
                yield
                hgrn_block(i, sl, pf3, hgV, Vblks[sl], scTs[sl], mixT3, km)
                yield
                KM = [(km, 0), (km, 1, 0), (km, 1, 1), (km, 2)]
                for hf in range(2):
                    b, pb, pk = bank()
                    MMG([(pb[:, :], mixT3[:, m, :], woutv[:, m, hf * 512:(hf + 1) * 512], m == 0, m == 7, None) for m in range(8)],
                        r=KM + WOUT, w=[pk])
                    STT(xt[sl][:, hf * 512:(hf + 1) * 512], xt[sl][:, hf * 512:(hf + 1) * 512], ALPHA, pb[:, :], ALU.mult, ALU.add, r=[pk, kx], w=[kx])
                    yield
                layer_norm(xt[sl], lnp[:, 0:D], lnp[:, D:2 * D], kx, tmp=lnts[sl])
                P.dma("sync", dst[0][i * 128:(i + 1) * 128, :], xt[sl], [kx], [(dst[1], i)], "st")
                yield
                if not (dbg and dbg.get("phases") == "A"):
                    xT32_3 = v3(xT32r, 8)
                    for hb in range(2):
                        bk, pb, pk = bank()
                        TRG([(pb[:, q * 128:(q + 1) * 128], xt[sl][:, (hb * 4 + q) * 128:(hb * 4 + q + 1) * 128]) for q in range(4)],
                            r=[kx, "cst"], w=[pk])
                        COPY("scalar", xT32_3[:, hb * 4:(hb + 1) * 4, :], v3(pb[:], 4), r=[pk], w=[("xT32", hb)])
                    bk, pb, pk = bank()
                    MMG([(pb[:, 0:NE], xT32_3[:, k, :], wr3[:, k, :], k == 0, k == 7, None) for k in range(8)], r=[("xT32", 0), ("xT32", 1), "wr"], w=[pk])
                    COPY("scalar", v3(rout[:], NB)[:, i, :], pb[:, 0:NE], r=[pk], w=[("logit", i)])

            nblk = dbg.get("nb", NB) if dbg else NB
            depth = dbg.get("depth", 2) if dbg else 2
            use_ls = (dbg.get("ls", 1) if dbg else 1)
            if use_ls:
                P.begin_capture()
            active, nxt = [], 0
            while nxt < nblk or active:
                if nxt < nblk and len(active) < depth:
                    active.append(blockgen(nxt))
                    nxt += 1
                for g_ in list(active):
                    try:
                        next(g_)
                    except StopIteration:
                        active.remove(g_)
            if use_ls:
                P.end_capture()

        def phase_B(l, src, dst):
            AF32.reset()
            ABF.reset()
            load_ln(l, 1)
            xt = [AF32.alloc(1024) for _ in range(2)]
            xT32 = AF32.alloc(1024)
            yacc = AF32.alloc(NBS * 1024)
            sg32 = [AF32.alloc(512) for _ in range(2)]
            wt_all = AF32.alloc(NBS * NE)
            rt = {nm: AF32.alloc(16) for nm in ["en", "sc", "sel", "eq", "sel2", "ge", "selm", "wsel"]}
            rs = AF32.alloc(32)
            xTm = ABF.alloc(8 * TSUP)
            hT = [ABF.alloc(2048) for _ in range(2)]
            xTm3 = v3(xTm, 8)
            yacc3 = v3(yacc, NBS)
            wt3 = v3(wt_all, NBS)
            wr3 = v3(wr_sb[:], 8)
            WG = lambda s_: v3(WBUF[:, (s_ * 3 + 0) * 4096:(s_ * 3 + 1) * 4096], 8)
            WU = lambda s_: v3(WBUF[:, (s_ * 3 + 1) * 4096:(s_ * 3 + 2) * 4096], 8)
            WD = lambda s_: v3(WBUF[:, (s_ * 3 + 2) * 4096:(s_ * 3 + 3) * 4096], 4)

            def load_expert(e):
                s_ = e % 2
                raise NotImplementedError("dense MoE path retired")

            for T in range(dbg.get("nT", S // TSUP) if dbg else S // TSUP):
                load_expert(0)
                P.enabled = (dbg is None) or (int(1) in dbg.get('bstages', range(10)))
                for blk in range(NBS):
                    i = T * NBS + blk
                    sl = blk % 2
                    kx = ("xt", sl)
                    P.enabled = (dbg is None) or (1 in dbg.get('bstages', range(10)))
                    P.dma("sync", xt[sl], src[0][i * 128:(i + 1) * 128, :], [(src[1], i)], [kx], "x")
                    xT32_3 = v3(xT32, 8)
                    for hb in range(2):
                        b, pb, pk = bank()
                        TRG([(pb[:, q * 128:(q + 1) * 128], xt[sl][:, (hb * 4 + q) * 128:(hb * 4 + q + 1) * 128]) for q in range(4)],
                            r=[kx, "cst"], w=[pk])
                        COPY("scalar", xT32_3[:, hb * 4:(hb + 1) * 4, :], v3(pb[:], 4), r=[pk], w=[("xT32", hb)])
                        COPY("vector", xTm3[:, hb * 4:(hb + 1) * 4, blk * 128:(blk + 1) * 128], v3(pb[:], 4), r=[pk], w=[("xTm", blk)])
                    b, pb, pk = bank()
                    if dbg and dbg.get("b1cut", 9) < 1:
                        P.enabled = False
                    MMG([(pb[:, 0:NE], xT32_3[:, k, :], wr3[:, k, :], k == 0, k == 7, None) for k in range(8)], r=[("xT32", 0), ("xT32", 1), "wr"], w=[pk])
                    R = rt
                    kr = "rt"
                    ACT(R["en"], pb[:, 0:NE], AF.Exp, scale=-1.0, r=[pk], w=[kr])
                    if dbg and dbg.get("b1cut", 9) < 2:
                        P.enabled = False
                    TS("vector", R["en"], R["en"], 1.0, None, ALU.add, r=[kr], w=[kr])
                    RECIP(R["sc"], R["en"], r=[kr], w=[kr])
                    TT("vector", R["sel"], R["sc"], rb_bc[:], ALU.add, r=[kr, "rb"], w=[kr])
                    sel3 = v3(R["sel"], 4)
                    m1, m2, gsum, gmax, ing, wsum, rws = rs[:, 0:4], rs[:, 4:8], rs[:, 8:12], rs[:, 12:13], rs[:, 16:20], rs[:, 20:21], rs[:, 21:22]
                    P.op("vector", lambda e, sel3=sel3, m1=m1: e.tensor_reduce(out=m1, in_=sel3, axis=AX.X, op=ALU.max), [kr], [kr])
                    TT("vector", v3(R["eq"], 4), sel3, bc(m1.unsqueeze(2), [128, 4, 4]), ALU.is_equal, r=[kr], w=[kr])
                    STT(R["sel2"], R["eq"], -1.0e9, R["sel"], ALU.mult, ALU.add, r=[kr], w=[kr])
                    P.op("vector", lambda e, R=R, m2=m2: e.tensor_reduce(out=m2, in_=v3(R["sel2"], 4), axis=AX.X, op=ALU.max), [kr], [kr])
                    TT("vector", gsum, m1, m2, ALU.add, r=[kr], w=[kr])
                    P.op("vector", lambda e, gsum=gsum, gmax=gmax: e.tensor_reduce(out=gmax, in_=gsum, axis=AX.X, op=ALU.max), [kr], [kr])
                    TS("vector", ing, gsum, gmax, None, ALU.is_equal, r=[kr], w=[kr])
                    TT("vector", v3(R["ge"], 4), sel3, bc(m2.unsqueeze(2), [128, 4, 4]), ALU.is_ge, r=[kr], w=[kr])
                    TT("vector", v3(R["selm"], 4), v3(R["ge"], 4), bc(ing.unsqueeze(2), [128, 4, 4]), ALU.mult, r=[kr], w=[kr])
                    TT("vector", R["wsel"], R["sc"], R["selm"], ALU.mult, r=[kr], w=[kr])
                    P.op("vector", lambda e, R=R, wsum=wsum: e.tensor_reduce(out=wsum, in_=R["wsel"], axis=AX.X, op=ALU.add), [kr], [kr])
                    RECIP(rws, wsum, r=[kr], w=[kr])
                    TS("vector", wt3[:, blk, :], R["wsel"], rws, None, ALU.mult, r=[kr], w=[("wt", blk)])
                P.enabled = (dbg is None) or (int(2) in dbg.get('bstages', range(10)))
                pend = None

                def down(e, sub, hs):
                    s_ = e % 2
                    hT3 = v3(hT[hs], 4)
                    for b4 in range(4):
                        blk = sub * 4 + b4
                        for hf in range(2):
                            b, pb, pk = bank()
                            MMG([(pb[:, :], hT3[:, jc, b4 * 128:(b4 + 1) * 128], WD(s_)[:, jc, hf * 512:(hf + 1) * 512], jc == 0, jc == 3, None) for jc in range(4)],
                                r=[("hT", hs, jc) for jc in range(4)] + [("W", s_ * 3 + 2)], w=[pk])
                            ysl = yacc3[:, blk, hf * 512:(hf + 1) * 512]
                            if e == 0:
                                TS("vector", ysl, pb[:, :], wt3[:, blk, e:e + 1], None, ALU.mult, r=[pk, ("wt", blk)], w=[("yacc", blk, hf)])
                            else:
                                STT(ysl, pb[:, :], wt3[:, blk, e:e + 1], ysl, ALU.mult, ALU.add, r=[pk, ("wt", blk), ("yacc", blk, hf)], w=[("yacc", blk, hf)])
                n_unit = 0
                for e in range(NE):
                    s_ = e % 2
                    for sub in range(TSUP // 512):
                        hs = n_unit % 2
                        hT3 = v3(hT[hs], 4)
                        for jc in range(4):
                            bg, pbg, pkg = bank()
                            bu, pbu, pku = bank()
                            MMG([(pbg[:, :], WG(s_)[:, k, jc * 128:(jc + 1) * 128], xTm3[:, k, sub * 512:(sub + 1) * 512], k == 0, k == 7, None) for k in range(8)],
                                r=[("W", s_ * 3 + 0)] + [("xTm", sub * 4 + q) for q in range(4)], w=[pkg])
                            MMG([(pbu[:, :], WU(s_)[:, k, jc * 128:(jc + 1) * 128], xTm3[:, k, sub * 512:(sub + 1) * 512], k == 0, k == 7, None) for k in range(8)],
                                r=[("W", s_ * 3 + 1)] + [("xTm", sub * 4 + q) for q in range(4)], w=[pku])
                            sgb = sg32[jc % 2]
                            ACT(sgb, pbg[:, :], AF.Silu, r=[pkg], w=[("sg", jc % 2)])
                            TT("vector", hT3[:, jc, :], sgb, pbu[:, :], ALU.mult, r=[("sg", jc % 2), pku], w=[("hT", hs, jc)])
                        if pend is not None:
                            down(*pend)
                        if sub == 0 and e + 1 < NE:
                            load_expert(e + 1)
                        pend = (e, sub, hs)
                        n_unit += 1
                down(*pend)
                P.enabled = (dbg is None) or (int(3) in dbg.get('bstages', range(10)))
                for blk in range(NBS):
                    i = T * NBS + blk
                    sl = blk % 2
                    kx = ("xt", sl)
                    P.dma("sync", xt[sl], src[0][i * 128:(i + 1) * 128, :], [(src[1], i)], [kx], "x")
                    for hf in range(2):
                        STT(xt[sl][:, hf * 512:(hf + 1) * 512], xt[sl][:, hf * 512:(hf + 1) * 512], ALPHA, yacc3[:, blk, hf * 512:(hf + 1) * 512], ALU.mult, ALU.add,
                            r=[kx, ("yacc", blk, hf)], w=[kx])
                    layer_norm(xt[sl], lnp[:, 0:D], lnp[:, D:2 * D], kx)
                    P.dma("sync", dst[0][i * 128:(i + 1) * 128, :], xt[sl], [kx], [(dst[1], i)], "st")

        def phase_B2(l, src, dst):
            AF32.reset()
            ABF.reset()
            load_ln(l, 1)
            nbB = dbg.get("nb", NB) if dbg else NB
            nj = (nbB * 128 + 4 * 511) // 512
            xr = [AF32.alloc(XW) for _ in range(2)]
            ing_all, posb_all, base_all, wq_all, cnt_all = (AF32.alloc(NB * 4) for _ in range(5))
            ing3, posb3, base3, wq3, cnt3 = v3(ing_all, NB), v3(posb_all, NB), v3(base_all, NB), v3(wq_all, NB), v3(cnt_all, NB)
            run = runp[:]
            slot_f = AF32.alloc(NB)
            gl = AF32.alloc(64)
            tmp4 = [AF32.alloc(4) for _ in range(2)]
            widx_f = AF32.alloc(NJ * 8)
            cmp1 = AF32.alloc(32)
            cmp2 = AF32.alloc(NJ * 4)
            xsr = [AF32.alloc(XW) for _ in range(4)]
            yacc = AF32.alloc(4096)
            sg32 = [AF32.alloc(512) for _ in range(2)]
            wqs = [AF32.alloc(16) for _ in range(2)]
            yg = [xsr[0][:, 0:1024], xsr[1][:, 0:1024]]
            xTm = ABF.alloc(8 * 512)
            hT = [ABF.alloc(2048) for _ in range(2)]
            xTm3 = v3(xTm, 8)
            yacc3 = v3(yacc, 4)
            wr3 = v3(wr_sb[:], 8)
            for sl_ in range(2):
                MEMSET("gpsimd", xr[sl_][:, 1024:XW], 0.0, [("xrpad", sl_)])
                MEMSET("gpsimd", xsr[sl_][:, 1024:XW], 0.0, [("xsrpad", sl_)])

            WG = lambda s_: WBUF[:, (s_ * 3 + 0) * 4096:(s_ * 3 + 1) * 4096]
            WU = lambda s_: WBUF[:, (s_ * 3 + 1) * 4096:(s_ * 3 + 2) * 4096]
            WD = lambda s_: WBUF[:, (s_ * 3 + 2) * 4096:(s_ * 3 + 3) * 4096]

            def load_w(j, q, s_):
                for m_, (dst_, srcw) in enumerate([(WG(s_), wgr), (WU(s_), wur), (WD(s_), wdr)]):
                    for hh in range(2):
                        col = (j * 4 + q) * 2 + hh
                        P.dma("gpsimd", None, None, ["widx"], [("W", s_ * 3 + m_, hh)], "w",
                              fn=(lambda e, dst_=dst_, srcw=srcw, hh=hh, col=col: e.indirect_dma_start(
                                  out=dst_[:, hh * 2048:(hh + 1) * 2048], out_offset=None, in_=srcw,
                                  in_offset=bass.IndirectOffsetOnAxis(ap=widx_i[:, col:col + 1], axis=0))))

            use_ls = (dbg.get("ls", 1) if dbg else 1)
            if use_ls:
                P.begin_capture()
            nbq = nbB
            W16 = nbq * NE
            rA, rB, rC, rD = yacc[:, 0:W16], yacc[:, 512:512 + W16], yacc[:, 1024:1024 + W16], yacc[:, 1536:1536 + W16]
            m1 = yacc[:, 2048:2048 + nbq * 4]
            m2 = yacc[:, 2176:2176 + nbq * 4]
            gsum = yacc[:, 2304:2304 + nbq * 4]
            gmax = yacc[:, 2432:2432 + nbq]
            wsum = yacc[:, 2464:2464 + nbq]
            KR = "routtmp"
            LGK = [("logit", b) for b in range(nbq)]
            ACT(rA, rout[:, 0:W16], AF.Exp, scale=-1.0, r=LGK, w=[KR])
            TS("vector", rA, rA, 1.0, None, ALU.add, r=[KR], w=[KR])
            RECIP(rA, rA, r=[KR], w=[KR])
            TT("vector", v3(rB, nbq), v3(rA, nbq), bc(rb_bc[:].unsqueeze(1), [128, nbq, NE]), ALU.add, r=[KR, "rb"], w=[KR])
            sel4 = v3(rB, nbq * 4)
            P.op("vector", lambda e: e.tensor_reduce(out=m1, in_=sel4, axis=AX.X, op=ALU.max), [KR], [KR], cost=700.0)
            TT("vector", v3(rC, nbq * 4), sel4, bc(m1.unsqueeze(2), [128, nbq * 4, 4]), ALU.is_equal, r=[KR], w=[KR])
            STT(rC, rC, -1.0e9, rB, ALU.mult, ALU.add, r=[KR], w=[KR])
            P.op("vector", lambda e: e.tensor_reduce(out=m2, in_=v3(rC, nbq * 4), axis=AX.X, op=ALU.max), [KR], [KR], cost=700.0)
            TT("vector", gsum, m1, m2, ALU.add, r=[KR], w=[KR])
            P.op("vector", lambda e: e.tensor_reduce(out=gmax, in_=v3(gsum, nbq), axis=AX.X, op=ALU.max), [KR], [KR])
            TT("vector", ing3[:, 0:nbq, :], v3(gsum, nbq), bc(gmax.unsqueeze(2), [128, nbq, 4]), ALU.is_equal, r=[KR], w=[KR, "ingall"])
            TT("vector", v3(rD, nbq * 4), sel4, bc(m2.unsqueeze(2), [128, nbq * 4, 4]), ALU.is_ge, r=[KR], w=[KR])
            TT("vector", v3(rD, nbq * 4), v3(rD, nbq * 4), bc(ing_all[:, 0:nbq * 4].unsqueeze(2), [128, nbq * 4, 4]), ALU.mult, r=[KR, "ingall"], w=[KR])
            TT("vector", rD, rA, rD, ALU.mult, r=[KR], w=[KR])
            P.op("vector", lambda e: e.tensor_reduce(out=wsum, in_=v3(rD, nbq), axis=AX.X, op=ALU.add), [KR], [KR], cost=700.0)
            RECIP(wsum, wsum, r=[KR], w=[KR])
            TT("vector", v3(rD, nbq), v3(rD, nbq), bc(wsum.unsqueeze(2), [128, nbq, NE]), ALU.mult, r=[KR], w=[KR])
            P.op("vector", lambda e: e.tensor_reduce(out=wq3[:, 0:nbq, :], in_=rD.rearrange("p (b g q) -> p b q g", b=nbq, g=4), axis=AX.X, op=ALU.add),
                 [KR], [("wq", b) for b in range(nbq)], cost=700.0)
            bk2, pb2, pk2 = bank()
            MMG([(pb2[:, 0:nbq * 4], tri, ing_all[:, 0:nbq * 4], True, True, None), (pb2[:, 128:128 + nbq * 4], ones128, ing_all[:, 0:nbq * 4], True, True, None)],
                r=["ingall", "cst"], w=[pk2])
            COPY("vector", posb_all[:, 0:nbq * 4], pb2[:, 0:nbq * 4], r=[pk2], w=[("posb", b) for b in range(nbq)])
            COPY("vector", cnt_all[:, 0:nbq * 4], pb2[:, 128:128 + nbq * 4], r=[pk2], w=["cntall"])
            MEMSET("vector", base3[:, 0, :], 0.0, ["baseall"])
            for b in range(1, nbq):
                TT("vector", base3[:, b, :], base3[:, b - 1, :], cnt3[:, b - 1, :], ALU.add, r=["baseall", "cntall"], w=["baseall"])
            TT("vector", run, base3[:, nbq - 1, :], cnt3[:, nbq - 1, :], ALU.add, r=["baseall", "cntall"], w=["run"])
            nbk, incl, pst, gidf, tg = gl[:, 0:4], gl[:, 4:8], gl[:, 8:12], gl[:, 16:16 + NJ], gl[:, 32:32 + NJ]
            KG = "glob"
            TT("vector", v3(cmp1, 4), bc(run.unsqueeze(2), [128, 4, 8]), bc(thr.unsqueeze(1), [128, 4, 8]), ALU.is_gt, r=["run", "cst"], w=[KG])
            P.op("vector", lambda e: e.tensor_reduce(out=nbk, in_=v3(cmp1, 4), axis=AX.X, op=ALU.add), [KG], [KG])
            COPY("vector", incl[:, 0:1], nbk[:, 0:1], r=[KG], w=[KG])
            for g in range(1, 4):
                TT("vector", incl[:, g:g + 1], incl[:, g - 1:g], nbk[:, g:g + 1], ALU.add, r=[KG], w=[KG])
            TT("vector", pst, incl, nbk, ALU.subtract, r=[KG], w=[KG])
            TS("vector", pst, pst, 512.0, None, ALU.mult, r=[KG], w=[KG])
            TT("vector", v3(cmp2, NJ), bc(incl.unsqueeze(1), [128, NJ, 4]), bc(jiota.unsqueeze(2), [128, NJ, 4]), ALU.is_le, r=[KG, "cst"], w=[KG])
            P.op("vector", lambda e: e.tensor_reduce(out=gidf, in_=v3(cmp2, NJ), axis=AX.X, op=ALU.add), [KG], [KG])
            TS("vector", gidf, gidf, 3.0, None, ALU.min, r=[KG], w=[KG])
            TS("vector", tg, gidf, 1024.0, iota2p, ALU.mult, ALU.add, r=[KG, "cst"], w=[KG])
            wf4 = widx_f.rearrange("p (j q h) -> p j q h", j=NJ, q=4)
            for q in range(4):
                for hh in range(2):
                    TS("vector", wf4[:, :, q, hh], tg, float((l * NE + q) * 256 + hh), None, ALU.add, r=[KG], w=[KG])
            COPY("vector", widx_i[:], widx_f, r=[KG], w=["widx"])
            load_w(0, 0, 0)
            for b in range(nbB):
                t4 = tmp4[b % 2]
                kt = ("tmp4", b % 2)
                TT("vector", t4, base3[:, b, :], posb3[:, b, :], ALU.add, r=["baseall", ("posb", b)], w=[kt])
                TT("vector", t4, t4, pst, ALU.add, r=[kt, KG], w=[kt])
                TT("vector", t4, t4, ing3[:, b, :], ALU.mult, r=[kt, "ingall"], w=[kt])
                P.op("vector", lambda e, t4=t4, b=b: e.tensor_reduce(out=slot_f[:, b:b + 1], in_=t4, axis=AX.X, op=ALU.add), [kt], [("slotf", b)])
            COPY("vector", slot_i[:, 0:nbB], slot_f[:, 0:nbB], r=[("slotf", b) for b in range(nbB)], w=["slot_i"])
            p2x = [(xr[0], ("xr", 0), ("xrpad", 0)), (xr[1], ("xr", 1), ("xrpad", 1)), (xsr[0], ("xsr", 0), ("xsrpad", 0)), (xsr[1], ("xsr", 1), ("xsrpad", 1))]
            for b in range(nbB):
                row, kx, kpad = p2x[b % 4]
                P.dma("sync", row[:, 0:1024], src[0][b * 128:(b + 1) * 128, :], [(src[1], b)], [kx], "x")
                COPY("gpsimd", row[:, 1024:1028], wq3[:, b, :], r=[("wq", b)], w=[kpad])
                P.dma("gpsimd", None, None, [kx, kpad, "slot_i"] + [("xsz", j_) for j_ in range(NJ)], [("xs", b)], "ix",
                      fn=(lambda e, row=row, b=b: e.indirect_dma_start(out=xs_d, out_offset=bass.IndirectOffsetOnAxis(ap=slot_i[:, b:b + 1], axis=0),
                                                                    in_=row[:, :], in_offset=None)))
            XSK = [("xs", b) for b in range(nbB)]
            if use_ls:
                P.end_capture()

            def down(j, q, s_, hs):
                hT3 = v3(hT[hs], 4)
                wd3 = v3(WD(s_), 4)
                wq_v = v3(wqs[j % 2], 4)
                for b4 in range(4):
                    for hf in range(2):
                        bk, pb, pk = bank()
                        MMG([(pb[:, :], hT3[:, jc, b4 * 128:(b4 + 1) * 128], wd3[:, jc, hf * 512:(hf + 1) * 512], jc == 0, jc == 3, None) for jc in range(4)],
                            r=[("hT", hs, jc) for jc in range(4)] + [("W", s_ * 3 + 2, 0), ("W", s_ * 3 + 2, 1)], w=[pk])
                        ysl = yacc3[:, b4, hf * 512:(hf + 1) * 512]
                        if q == 0:
                            TS("vector", ysl, pb[:, :], wq_v[:, b4, q:q + 1], None, ALU.mult, r=[pk, ("wqs", j % 2, b4)], w=[("yacc", b4, hf)])
                        else:
                            STT(ysl, pb[:, :], wq_v[:, b4, q:q + 1], ysl, ALU.mult, ALU.add, r=[pk, ("wqs", j % 2, b4), ("yacc", b4, hf)], w=[("yacc", b4, hf)])
                if q == 3:
                    P.dma("sync", ys_d[j * 512:(j + 1) * 512, :].rearrange("(a p) d -> p a d", p=128), yacc3,
                          [("yacc", b4, hf) for b4 in range(4) for hf in range(2)], [("ys", j)], "st")

            pend = None
            n_unit = 0
            for j in range(nj):
                for b4 in range(4):
                    P.dma("sync", xsr[b4], xs_d[j * 512 + b4 * 128:j * 512 + (b4 + 1) * 128, :], XSK + [("xsz", j)], [("xsr", b4), ("xsrpad", b4)], "x")
                    for hb in range(2):
                        bk, pb, pk = bank()
                        TRG([(pb[:, q_ * 128:(q_ + 1) * 128], xsr[b4][:, (hb * 4 + q_) * 128:(hb * 4 + q_ + 1) * 128]) for q_ in range(4)],
                            r=[("xsr", b4), "cst"], w=[pk])
                        COPY("scalar" if hb == 0 else "vector", xTm3[:, hb * 4:(hb + 1) * 4, b4 * 128:(b4 + 1) * 128], v3(pb[:], 4), r=[pk], w=[("xTm", b4, hb)])
                    COPY("gpsimd", v3(wqs[j % 2], 4)[:, b4, :], xsr[b4][:, 1024:1028], r=[("xsr", b4), ("xsrpad", b4)], w=[("wqs", j % 2, b4)])
                XTMK = [("xTm", b4, hb) for b4 in range(4) for hb in range(2)]
                for q in range(4):
                    s_ = n_unit % 2
                    hs = n_unit % 2
                    hT3 = v3(hT[hs], 4)
                    wg3, wu3 = v3(WG(s_), 8), v3(WU(s_), 8)
                    for jc in range(4):
                        bg, pbg, pkg = bank()
                        bu, pbu, pku = bank()
                        MMG([(pbg[:, :], wg3[:, k, jc * 128:(jc + 1) * 128], xTm3[:, k, :], k == 0, k == 7, None) for k in range(8)],
                            r=[("W", s_ * 3 + 0, 0), ("W", s_ * 3 + 0, 1)] + XTMK, w=[pkg])
                        MMG([(pbu[:, :], wu3[:, k, jc * 128:(jc + 1) * 128], xTm3[:, k, :], k == 0, k == 7, None) for k in range(8)],
                            r=[("W", s_ * 3 + 1, 0), ("W", s_ * 3 + 1, 1)] + XTMK, w=[pku])
                        sgb = sg32[jc % 2]
                        ACT(sgb, pbg[:, :], AF.Silu, r=[pkg], w=[("sg", jc % 2)])
                        TT("vector", hT3[:, jc, :], sgb, pbu[:, :], ALU.mult, r=[("sg", jc % 2), pku], w=[("hT", hs, jc)])
                    if pend is not None:
                        down(*pend)
                    nxt = n_unit + 1
                    if nxt < nj * 4:
                        load_w(nxt // 4, nxt % 4, nxt % 2)
                    pend = (j, q, s_, hs)
                    n_unit += 1
            down(*pend)
            P.barrier()
            if l + 1 < n_layers:
                load_A_weights(l + 1)
                prefetched.add(l + 1)
            if use_ls:
                P.begin_capture()
            YSK = [("ys", j) for j in range(nj)]
            AF32.reset()
            NS4 = 7
            p4x = [AF32.alloc(1024) for _ in range(NS4)]
            p4y = [AF32.alloc(1024) for _ in range(NS4)]
            ln4 = [AF32.alloc(18) for _ in range(NS4)]
            def p4_issue(b):
                sl = b % NS4
                P.dma("sync", p4x[sl], src[0][b * 128:(b + 1) * 128, :], [(src[1], b)], [("p4x", sl)], "x")
                P.dma("gpsimd", None, None, YSK + ["slot_i"], [("p4y", sl)], "ix",
                      fn=(lambda e, sl=sl, b=b: e.indirect_dma_start(out=p4y[sl], out_offset=None, in_=ys_d[0:nj * 512, :],
                                                                  in_offset=bass.IndirectOffsetOnAxis(ap=slot_i[:, b:b + 1], axis=0))))
            for b in range(min(NS4 - 1, nbB)):
                p4_issue(b)
            for b in range(nbB):
                if b + NS4 - 1 < nbB:
                    p4_issue(b + NS4 - 1)
                sl = b % NS4
                kx = ("p4x", sl)
                xz = p4x[sl]
                for hf in range(2):
                    STT(xz[:, hf * 512:(hf + 1) * 512], xz[:, hf * 512:(hf + 1) * 512], ALPHA, p4y[sl][:, hf * 512:(hf + 1) * 512], ALU.mult, ALU.add,
                        r=[kx, ("p4y", sl)], w=[kx])
                layer_norm(xz, lnp[:, 0:D], lnp[:, D:2 * D], kx, eng_g="vector", eng_b="gpsimd", tmp=ln4[sl])
                P.dma("sync", dst[0][b * 128:(b + 1) * 128, :], xz, [kx], [(dst[1], b)], "st")
            if use_ls:
                P.end_capture()

        phases = dbg.get("phases") if dbg else None
        for l in range(n_layers):
            srcA = (x_in, "x_in") if l == 0 else (xb, "xb")
            lastA = phases is not None and phases == "A" and l == n_layers - 1
            dstA = (out, "out") if lastA else (xa, "xa")
            phase_A(l, srcA, dstA)
            P.barrier()
            if lastA:
                break
            dstB = (out, "out") if l == n_layers - 1 else (xb, "xb")
            (phase_B if (dbg and dbg.get("dense")) else phase_B2)(l, (xa, "xa"), dstB)
            P.barrier()
        P.emit(nc, st)
    return nc


def _col_perm():
    cols = []
    for j in range(4):
        cols += list(range(j * 64, j * 64 + 64)) + list(range((4 + j) * 64, (4 + j) * 64 + 64))
    cols += list(range(512, 640))
    cols += list(range(768, 1024))
    cols += list(range(1024, 1280))
    cols += list(range(1536, 1792))
    cols += list(range(1792, 2048))
    cols += list(range(640, 768))
    cols += list(range(1280, 1536))
    return np.array(cols)


def _row_perm():
    rows = []
    for j in range(4):
        rows += list(range(j * 64, j * 64 + 64)) + list(range((4 + j) * 64, (4 + j) * 64 + 64))
    rows += list(range(512, 1024))
    return np.array(rows)


def _t5_bucket(dist):
    d = np.maximum(dist, 0)
    large = 16 + (np.log(np.maximum(d, 1).astype(np.float32) / 16) / math.log(128 / 16) * 16).astype(np.int32)
    large = np.minimum(large, 31)
    return np.where(d < 16, d, large)


def _constants():
    c = np.zeros((128, 1024), np.float32)
    c[:, 0:128] = np.eye(128, dtype=np.float32)
    s_ = np.arange(128)[:, None]
    t_ = np.arange(128)[None, :]
    c[:, 128:256] = ((s_ <= t_) & (s_ // 16 == t_ // 16)).astype(np.float32)
    c[:, 256:384] = (t_ % 16 != 0).astype(np.float32)
    c[:, 384:512] = (s_ // 64 == t_ // 64).astype(np.float32) / 64.0
    c[:, 512:520] = (s_ // 16 == np.arange(8)[None, :]).astype(np.float32)
    c[:, 640:768] = (s_ < t_).astype(np.float32)
    c[:, 768:896] = 1.0
    c[:, 896] = 2.0 * np.arange(128)
    c[:, 897:905] = 512.0 * np.arange(8)[None, :]
    c[:, 905:905 + NJ] = np.arange(NJ)[None, :]
    return c


def _rows(w, nk):
    w = np.asarray(w, dtype=np.float32)
    n = w.shape[-1]
    w = w.reshape(L, NE, nk, 128, n).transpose(0, 1, 3, 2, 4)
    return np.ascontiguousarray(w).reshape(L * NE * 128 * 2, 2048)


def _prep(inputs):
    f = lambda a: np.ascontiguousarray(np.asarray(a, dtype=np.float32))
    cp, rp = _col_perm(), _row_perm()
    w_in = f(np.asarray(inputs["w_in"])[:, :, cp])
    b_in = np.asarray(inputs["b_in"], dtype=np.float32)[:, cp]
    w_out = f(np.asarray(inputs["w_out"])[:, rp, :])
    bfm = f(b_in[:, :NFM * 128].reshape(L, NFM, 128).transpose(2, 0, 1).reshape(128, L * NFM))
    btm = f(b_in[:, NFM * 128:])
    lbl = f(np.asarray(inputs["hgrn_lb_logits"], dtype=np.float32).reshape(L, 2, 128).transpose(2, 0, 1).reshape(128, L * 2))
    nw = np.asarray(inputs["hgrn_norm"], dtype=np.float32)
    normw = f(np.tile(nw, (1, 2)).T)
    sk = np.asarray(inputs["attn_sinks"], dtype=np.float32)
    sinks = f(np.repeat(sk.reshape(L, 2, 4), 64, axis=1).transpose(1, 0, 2).reshape(128, L * 4))
    lnp = f(np.stack([inputs["ln1_g"], inputs["ln1_b"], inputs["ln2_g"], inputs["ln2_b"]], axis=1))
    rb = f(np.asarray(inputs["router_bias"]).reshape(1, NE))
    rel = np.asarray(inputs["rel_bias"], dtype=np.float32)
    j = np.arange(128)[:, None]
    i = np.arange(128)[None, :]
    biasT = np.zeros((128, 2, 2, 4, 128), np.float32)
    maskf = np.zeros((128, 2, 2, 4, 128), np.float32)
    for pv in range(2):
        dist = (i + 128 - j) if pv == 0 else (i - j)
        bk = _t5_bucket(dist)
        inw = (dist >= 0) & (dist < 128)
        for g in range(2):
            for j4 in range(4):
                biasT[:, pv, g, j4, :] = rel[bk, g * 4 + j4]
                maskf[:, pv, g, j4, :] = inw
    shared = {
        "w_in": w_in, "w_out": w_out, "w_mem": f(inputs["w_mem_kv"]),
        "wgr": _rows(inputs["w_gate"], 8), "wur": _rows(inputs["w_up"], 8), "wdr": _rows(inputs["w_down"], 4),
        "w_router": f(inputs["w_router"]), "cst": _constants(),
        "biasT": f(biasT.reshape(128, 2048)), "maskf": f(maskf.reshape(128, 2048)),
        "bfm": bfm, "btm": btm, "lbl": lbl, "normw": normw, "sinks": sinks, "lnp": lnp, "rbias": rb,
    }
    return shared


_NC_CACHE = {}


def kernel(**inputs):
    shared = _prep(inputs)
    x = np.asarray(inputs["x"], dtype=np.float32)
    mem = np.asarray(inputs["mem"], dtype=np.float32)
    if "nc" not in _NC_CACHE:
        _NC_CACHE["nc"] = build_program()
    nc = _NC_CACHE["nc"]
    in_maps = []
    for c in range(NCORES):
        m = dict(shared)
        m["x"] = np.ascontiguousarray(x[c])
        m["mem"] = np.ascontiguousarray(mem[c])
        in_maps.append(m)
    res = run_bass_kernel_spmd(nc, in_maps, core_ids=list(range(NCORES)))
    return np.stack([r["out"] for r in res.results], axis=0)
```
